# Optimizing a Trainium2 kernel written in Bass

```python
import math
import jax
import jax.numpy as jnp
from jax import lax
import numpy as np


D_MODEL = 1024
BATCH = 4
SEQ = 4096
DEPTH = 2

CHUNK = 64
Q_BLOCK = 128
N_BRANCH = 4
BRANCH_WIDTH = D_MODEL // N_BRANCH
HEAD_DIM = 64
N_HEADS = BRANCH_WIDTH // HEAD_DIM
IDX_HEADS = 4
IDX_DIM = 32
TOPK_MAX = 256
RET_DECAY_BASE = 5.0
S5_GROUP = 16
S5_GROUPS = BRANCH_WIDTH // S5_GROUP
S5_STATE = 64
S5_DT_MIN = 1e-3
S5_DT_MAX = 1e-1
MLA_Q_RANK = D_MODEL // 4
MLA_KV_RANK = D_MODEL // 8
MLA_NOPE = 64
MLA_ROPE = 32
MLA_V = 64
ROPE_THETA = 10000.0
D_FF = 2816
LN_EPS = 1e-5
RMS_EPS = 1e-6
DN_ALPHA = (2 * DEPTH) ** 0.25
DN_BETA = (8 * DEPTH) ** -0.25

IN_WIDTHS = (
    N_HEADS * HEAD_DIM,
    HEAD_DIM,
    HEAD_DIM,
    IDX_HEADS * IDX_DIM,
    IDX_DIM,
    IDX_HEADS,
    BRANCH_WIDTH,
    BRANCH_WIDTH,
    BRANCH_WIDTH,
    BRANCH_WIDTH,
    BRANCH_WIDTH,
    MLA_Q_RANK,
    MLA_KV_RANK,
    MLA_ROPE,
    N_BRANCH * D_MODEL,
)
N_IN = sum(IN_WIDTHS)
IN_SPLITS = tuple(int(v) for v in np.cumsum(IN_WIDTHS)[:-1])

kernel_name = 'hybrid_chunk_causal_dsa_retention_s5_mla_block'


def _layer_norm(x, g, b):
    xf = x.astype(jnp.float32)
    mu = jnp.mean(xf, axis=-1, keepdims=True)
    var = jnp.mean(jnp.square(xf - mu), axis=-1, keepdims=True)
    return (xf - mu) * lax.rsqrt(var + LN_EPS) * g + b


def _rms_norm(x, g):
    xf = x.astype(jnp.float32)
    return xf * lax.rsqrt(jnp.mean(jnp.square(xf), axis=-1, keepdims=True) + RMS_EPS) * g


def _swiglu(x, wi, wo):
    a, b = jnp.split(x @ wi, 2, axis=-1)
    return (jax.nn.silu(a) * b) @ wo


def _chunk_limit(pos):
    return (pos // CHUNK + 1) * CHUNK


def _alibi_slopes(h):
    return jnp.exp2(-8.0 * jnp.arange(1, h + 1, dtype=jnp.float32) / h)


def _rope(x, pos):
    half = x.shape[-1] // 2
    freqs = ROPE_THETA ** (-jnp.arange(half, dtype=jnp.float32) / half)
    ang = pos[:, None] * freqs[None, :]
    cos = jnp.cos(ang)[:, None, :]
    sin = jnp.sin(ang)[:, None, :]
    x1, x2 = x[..., :half], x[..., half:]
    return jnp.concatenate([x1 * cos - x2 * sin, x1 * sin + x2 * cos], axis=-1)


def _to_blocks(a):
    b, s = a.shape[:2]
    return jnp.moveaxis(a.reshape((b, s // Q_BLOCK, Q_BLOCK) + a.shape[2:]), 1, 0)


def _from_blocks(a):
    nb, b, qb = a.shape[:3]
    return jnp.moveaxis(a, 0, 1).reshape((b, nb * qb) + a.shape[3:])


def _dsa(q, k, v, iq, ik, iw):
    s_len = q.shape[1]
    topk = min(TOPK_MAX, s_len // 4)
    key_pos = jnp.arange(s_len, dtype=jnp.int32)
    slopes = _alibi_slopes(N_HEADS)
    scale = HEAD_DIM ** -0.5
    iw = iw.astype(jnp.float32) * (IDX_HEADS ** -0.5 * IDX_DIM ** -0.5)
    gather = jax.vmap(lambda tab, ix: tab[ix])

    def block(args):
        qb, iqb, iwb, blk = args
        qpos = blk * Q_BLOCK + jnp.arange(Q_BLOCK, dtype=jnp.int32)
        limit = _chunk_limit(qpos)
        admissible = key_pos[None, :] < limit[:, None]
        rel = jax.nn.relu(jnp.einsum('bqhc,bsc->bqhs', iqb, ik).astype(jnp.float32))
        iscore = jnp.einsum('bqhs,bqh->bqs', rel, iwb)
        iscore = jnp.where(admissible[None], iscore, -jnp.inf)
        _, idx = lax.top_k(iscore, topk)
        k_sel = gather(k, idx)
        v_sel = gather(v, idx)
        logits = jnp.einsum('bqhd,bqkd->bhqk', qb, k_sel).astype(jnp.float32) * scale
        dist = jnp.abs(qpos[None, :, None] - idx).astype(jnp.float32)
        logits = logits - slopes[None, :, None, None] * dist[:, None]
        valid = idx < limit[None, :, None]
        logits = jnp.where(valid[:, None], logits, -jnp.inf)
        p = jax.nn.softmax(logits, axis=-1)
        return jnp.einsum('bhqk,bqkd->bqhd', p.astype(v_sel.dtype), v_sel)

    nb = s_len // Q_BLOCK
    out = lax.map(block, (_to_blocks(q), _to_blocks(iq), _to_blocks(iw), jnp.arange(nb, dtype=jnp.int32)))
    return _from_blocks(out)


def _retention(q, k, v, g, gn_w):
    b, s_len, h, dk = q.shape
    dv = v.shape[-1]
    nc = s_len // CHUNK
    log_g = jnp.log1p(-jnp.exp2(-RET_DECAY_BASE - jnp.arange(h, dtype=jnp.float32)))
    n = jnp.arange(CHUNK, dtype=jnp.float32)
    decay_intra = jnp.exp(log_g[:, None, None] * jnp.abs(n[:, None] - n[None, :]))
    xi = jnp.exp(log_g[:, None] * (n + 1.0))
    zeta = jnp.exp(log_g[:, None] * (CHUNK - 1.0 - n))
    g_chunk = jnp.exp(log_g * CHUNK)

    def chunked(a):
        return a.reshape(b, nc, CHUNK, h, -1).transpose(0, 3, 1, 2, 4).astype(jnp.float32)

    qc = chunked(q)
    kc = chunked(k) * (dk ** -0.5)
    vc = chunked(v)
    scores = jnp.einsum('bhjnd,bhjmd->bhjnm', qc, kc) * decay_intra[:, None]
    intra = jnp.einsum('bhjnm,bhjme->bhjne', scores, vc)
    kv = jnp.einsum('bhjmd,bhjme->bhjde', kc * zeta[:, None, :, None], vc)

    def step(state, kv_j):
        return g_chunk[None, :, None, None] * state + kv_j, state

    _, prev = lax.scan(step, jnp.zeros((b, h, dk, dv), jnp.float32), jnp.moveaxis(kv, 2, 0))
    prev = jnp.moveaxis(prev, 0, 2)
    inter = jnp.einsum('bhjnd,bhjde->bhjne', qc * xi[:, None, :, None], prev)
    ret = (intra + inter).transpose(0, 2, 3, 1, 4).reshape(b, s_len, h, dv)
    mu = jnp.mean(ret, axis=-1, keepdims=True)
    var = jnp.mean(jnp.square(ret - mu), axis=-1, keepdims=True)
    ret = ((ret - mu) * lax.rsqrt(var + LN_EPS)).reshape(b, s_len, h * dv) * gn_w
    return jax.nn.silu(g.astype(jnp.float32)) * ret


def _s5(u, a_re, a_im, b_re, b_im, c_re, c_im, d, log_step, w_glu):
    b, s_len = u.shape[:2]
    uf = u.astype(jnp.float32).reshape(b, s_len, S5_GROUPS, S5_GROUP)
    a = lax.complex(a_re.astype(jnp.float32), a_im.astype(jnp.float32))
    dt = jnp.exp(log_step.astype(jnp.float32))[:, None]
    a_bar = jnp.exp(a * dt)
    b_mat = lax.complex(b_re.astype(jnp.float32), b_im.astype(jnp.float32))
    b_bar = ((a_bar - 1.0) / a)[..., None] * b_mat
    bu = jnp.einsum('bsgc,gpc->bsgp', uf.astype(jnp.complex64), b_bar)
    a_seq = jnp.broadcast_to(a_bar, bu.shape)

    def combine(e1, e2):
        a1, x1 = e1
        a2, x2 = e2
        return a2 * a1, a2 * x1 + x2

    _, states = lax.associative_scan(combine, (a_seq, bu), axis=1)
    c_mat = lax.complex(c_re.astype(jnp.float32), c_im.astype(jnp.float32))
    y = jnp.einsum('bsgp,gcp->bsgc', states, c_mat).real + d.astype(jnp.float32) * uf
    y = jax.nn.gelu(y.reshape(b, s_len, BRANCH_WIDTH))
    val, gate = jnp.split(y @ w_glu, 2, axis=-1)
    return val * jax.nn.sigmoid(gate)


def _mla(cq, ckv, kr, q_norm, kv_norm, w_uq, w_ukv):
    b, s_len = cq.shape[:2]
    pos = jnp.arange(s_len, dtype=jnp.float32)
    q = (_rms_norm(cq, q_norm) @ w_uq).reshape(b, s_len, N_HEADS, MLA_NOPE + MLA_ROPE)
    kv = (_rms_norm(ckv, kv_norm) @ w_ukv).reshape(b, s_len, N_HEADS, MLA_NOPE + MLA_V)
    k_nope, v = kv[..., :MLA_NOPE], kv[..., MLA_NOPE:]
    k_rope = _rope(kr[:, :, None, :].astype(jnp.float32), pos)
    q = jnp.concatenate([q[..., :MLA_NOPE], _rope(q[..., MLA_NOPE:], pos)], axis=-1)
    k = jnp.concatenate([k_nope, jnp.broadcast_to(k_rope, (b, s_len, N_HEADS, MLA_ROPE))], axis=-1)
    scale = (MLA_NOPE + MLA_ROPE) ** -0.5
    key_pos = jnp.arange(s_len, dtype=jnp.int32)

    def block(args):
        qb, blk = args
        qpos = blk * Q_BLOCK + jnp.arange(Q_BLOCK, dtype=jnp.int32)
        mask = key_pos[None, :] < _chunk_limit(qpos)[:, None]
        sc = jnp.einsum('bqhd,bkhd->bhqk', qb, k).astype(jnp.float32) * scale
        p = jax.nn.softmax(jnp.where(mask[None, None], sc, -jnp.inf), axis=-1)
        return jnp.einsum('bhqk,bkhd->bqhd', p.astype(v.dtype), v)

    nb = s_len // Q_BLOCK
    out = lax.map(block, (_to_blocks(q), jnp.arange(nb, dtype=jnp.int32)))
    return _from_blocks(out).reshape(b, s_len, N_HEADS * MLA_V)


def _mixer(x, w_in, ret_gn, s5_a_re, s5_a_im, s5_b_re, s5_b_im, s5_c_re, s5_c_im, s5_d,
           s5_log_step, s5_w_glu, mla_q_norm, mla_kv_norm, mla_w_uq, mla_w_ukv, w_branch, w_out):
    b, s_len, _ = x.shape
    h = x @ w_in
    (dq, dk, dv, iq, ik, iw, rq, rk, rv, rg, su, cq, ckv, kr, gates) = jnp.split(h, IN_SPLITS, axis=-1)
    o_a = _dsa(dq.reshape(b, s_len, N_HEADS, HEAD_DIM), dk, dv,
               iq.reshape(b, s_len, IDX_HEADS, IDX_DIM), ik, iw).reshape(b, s_len, BRANCH_WIDTH)
    o_b = _retention(rq.reshape(b, s_len, N_HEADS, HEAD_DIM), rk.reshape(b, s_len, N_HEADS, HEAD_DIM),
                     rv.reshape(b, s_len, N_HEADS, HEAD_DIM), rg, ret_gn)
    o_c = _s5(su, s5_a_re, s5_a_im, s5_b_re, s5_b_im, s5_c_re, s5_c_im, s5_d, s5_log_step, s5_w_glu)
    o_d = _mla(cq, ckv, kr, mla_q_norm, mla_kv_norm, mla_w_uq, mla_w_ukv)
    branches = jnp.stack([o_a.astype(jnp.float32), o_b.astype(jnp.float32),
                          o_c.astype(jnp.float32), o_d.astype(jnp.float32)], axis=2)
    up = jnp.einsum('bsnc,ncd->bsnd', branches, w_branch)
    gate = jax.nn.sigmoid(gates.reshape(b, s_len, N_BRANCH, D_MODEL).astype(jnp.float32))
    merged = jnp.sum(gate * up, axis=2)
    return merged @ w_out


def setup_inputs(seed: int = 0) -> dict:
    key = jax.random.key(seed)
    ks = jax.random.split(key, 22)
    nrm = jax.random.normal
    f32 = jnp.float32
    x = nrm(ks[0], (BATCH, SEQ, D_MODEL), f32)
    ln_g = 1.0 + 0.02 * nrm(ks[1], (DEPTH, 3, D_MODEL), f32)
    ln_b = 0.02 * nrm(ks[2], (DEPTH, 3, D_MODEL), f32)
    ffn_wi = nrm(ks[3], (DEPTH, 2, D_MODEL, 2 * D_FF), f32) * (D_MODEL ** -0.5 * DN_BETA)
    ffn_wo = nrm(ks[4], (DEPTH, 2, D_FF, D_MODEL), f32) * (D_FF ** -0.5 * DN_BETA)
    w_in = nrm(ks[5], (DEPTH, D_MODEL, N_IN), f32) * D_MODEL ** -0.5
    ret_gn = 1.0 + 0.02 * nrm(ks[6], (DEPTH, BRANCH_WIDTH), f32)
    s5_a_re = -0.5 * jnp.exp(0.02 * nrm(ks[7], (DEPTH, S5_GROUPS, S5_STATE), f32))
    s5_a_im = math.pi * jnp.arange(S5_STATE, dtype=f32) + 0.02 * nrm(ks[8], (DEPTH, S5_GROUPS, S5_STATE), f32)
    s5_b_re = nrm(ks[9], (DEPTH, S5_GROUPS, S5_STATE, S5_GROUP), f32) * (2 * S5_GROUP) ** -0.5
    s5_b_im = nrm(ks[10], (DEPTH, S5_GROUPS, S5_STATE, S5_GROUP), f32) * (2 * S5_GROUP) ** -0.5
    s5_c_re = nrm(ks[11], (DEPTH, S5_GROUPS, S5_GROUP, S5_STATE), f32) * (2 * S5_STATE) ** -0.5
    s5_c_im = nrm(ks[12], (DEPTH, S5_GROUPS, S5_GROUP, S5_STATE), f32) * (2 * S5_STATE) ** -0.5
    s5_d = nrm(ks[13], (DEPTH, S5_GROUPS, S5_GROUP), f32)
    s5_log_step = jax.random.uniform(ks[14], (DEPTH, S5_GROUPS), f32,
                                     minval=math.log(S5_DT_MIN), maxval=math.log(S5_DT_MAX))
    s5_w_glu = nrm(ks[15], (DEPTH, BRANCH_WIDTH, 2 * BRANCH_WIDTH), f32) * BRANCH_WIDTH ** -0.5
    mla_q_norm = 1.0 + 0.02 * nrm(ks[16], (DEPTH, MLA_Q_RANK), f32)
    mla_kv_norm = 1.0 + 0.02 * nrm(ks[17], (DEPTH, MLA_KV_RANK), f32)
    mla_w_uq = nrm(ks[18], (DEPTH, MLA_Q_RANK, N_HEADS * (MLA_NOPE + MLA_ROPE)), f32) * MLA_Q_RANK ** -0.5
    mla_w_ukv = nrm(ks[19], (DEPTH, MLA_KV_RANK, N_HEADS * (MLA_NOPE + MLA_V)), f32) * MLA_KV_RANK ** -0.5
    w_branch = nrm(ks[20], (DEPTH, N_BRANCH, BRANCH_WIDTH, D_MODEL), f32) * (BRANCH_WIDTH ** -0.5 * DN_BETA)
    w_out = nrm(ks[21], (DEPTH, D_MODEL, D_MODEL), f32) * (D_MODEL ** -0.5 * DN_BETA)
    return {'x': x, 'ln_g': ln_g, 'ln_b': ln_b, 'ffn_wi': ffn_wi, 'ffn_wo': ffn_wo, 'w_in': w_in,
            'ret_gn': ret_gn, 's5_a_re': s5_a_re, 's5_a_im': s5_a_im, 's5_b_re': s5_b_re,
            's5_b_im': s5_b_im, 's5_c_re': s5_c_re, 's5_c_im': s5_c_im, 's5_d': s5_d,
            's5_log_step': s5_log_step, 's5_w_glu': s5_w_glu, 'mla_q_norm': mla_q_norm,
            'mla_kv_norm': mla_kv_norm, 'mla_w_uq': mla_w_uq, 'mla_w_ukv': mla_w_ukv,
            'w_branch': w_branch, 'w_out': w_out}


def reference(x, ln_g, ln_b, ffn_wi, ffn_wo, w_in, ret_gn, s5_a_re, s5_a_im, s5_b_re, s5_b_im,
              s5_c_re, s5_c_im, s5_d, s5_log_step, s5_w_glu, mla_q_norm, mla_kv_norm, mla_w_uq,
              mla_w_ukv, w_branch, w_out):
    out_dtype = x.dtype
    for l in range(DEPTH):
        x = _layer_norm(DN_ALPHA * x + 0.5 * _swiglu(x, ffn_wi[l, 0], ffn_wo[l, 0]), ln_g[l, 0], ln_b[l, 0])
        mix = _mixer(x, w_in[l], ret_gn[l], s5_a_re[l], s5_a_im[l], s5_b_re[l], s5_b_im[l],
                     s5_c_re[l], s5_c_im[l], s5_d[l], s5_log_step[l], s5_w_glu[l], mla_q_norm[l],
                     mla_kv_norm[l], mla_w_uq[l], mla_w_ukv[l], w_branch[l], w_out[l])
        x = _layer_norm(DN_ALPHA * x + mix, ln_g[l, 1], ln_b[l, 1])
        x = _layer_norm(DN_ALPHA * x + 0.5 * _swiglu(x, ffn_wi[l, 1], ffn_wo[l, 1]), ln_g[l, 2], ln_b[l, 2])
    return x.astype(out_dtype)
```

```python
from contextlib import ExitStack
import math
import numpy as np
import concourse.bass as bass
import concourse.mybir as mybir
from concourse.bass_utils import run_bass_kernel_spmd

F32 = mybir.dt.float32
BF16 = mybir.dt.bfloat16
I32 = mybir.dt.int32
U32 = mybir.dt.uint32
AF = mybir.ActivationFunctionType
ALU = mybir.AluOpType
AX = mybir.AxisListType

NDMA = 24
SEQ = 4096
D = 1024
DFF = 2816
DEPTH = 2
ALPHA = (2 * DEPTH) ** 0.25
LN_EPS = 1e-5
RMS_EPS = 1e-6
NIN = 6340
NSM = 2244
BIG = 1.0e30


class Prog:
    ENG = ("pe", "act", "dve", "pool", "sp")

    def __init__(self, nc):
        self.nc = nc
        self.es = ExitStack()
        self.q = {e: [] for e in self.ENG}
        self.cnt = {e: 0 for e in self.ENG}
        self.sems = {}
        for e in self.ENG:
            self.sems[e] = self.es.enter_context(nc.semaphore("s_" + e))
        for i in range(NDMA):
            self.sems[("dma", i)] = self.es.enter_context(nc.semaphore("s_dma%d" % i))
        self.dma_cnt = [0] * NDMA
        self.dma_rr = 0
        self.known = {e: {} for e in self.ENG}
        self.last_w = {}
        self.readers = {}
        self.ntile = 0
        self.n_ops = 0
        self.sb_off = 16384 + 64
        self.sb_base = 16384 + 64
        self.sb_max = 0

    def sb(self, shape, dtype, name=None):
        self.ntile += 1
        name = name or ("t%d" % self.ntile)
        esz = 4 if dtype in (F32, I32, U32) else 2
        n = 1
        for s in shape[1:]:
            n *= s
        nbytes = (n * esz + 63) // 64 * 64
        off = self.sb_off
        self.sb_off += nbytes
        self.sb_max = max(self.sb_max, self.sb_off)
        assert self.sb_off <= 229376 - 512, ("SBUF overflow", self.sb_off)
        return self.nc.alloc_sbuf_tensor_at(name, list(shape), dtype, offset=off)

    def ps(self, shape, dtype, name=None):
        self.ntile += 1
        name = name or ("p%d" % self.ntile)
        return self.es.enter_context(self.nc.psum_tensor(name, list(shape), dtype))

    def persist(self):
        self.sb_base = self.sb_off

    def stage_begin(self):
        self.barrier()
        self.sb_off = self.sb_base

    def _deps(self, eng, reads, writes):
        w = {}

        def add(ev):
            if ev is None:
                return
            k, v = ev
            if k == "pe" and eng == "pe":
                return
            if self.known[eng].get(k, 0) >= v:
                return
            if w.get(k, 0) < v:
                w[k] = v

        for k in reads:
            add(self.last_w.get(k))
            if isinstance(k, tuple) and k[0] in ("ACC", "PW", "BB"):
                for ev in self.readers.get(k, {}).items():
                    if ev[0] != eng:
                        add(ev)
        for k in writes:
            add(self.last_w.get(k))
            for ev in self.readers.get(k, {}).items():
                add(ev)
        for k, v in w.items():
            self.known[eng][k] = v
        return list(w.items())

    def _commit(self, ev, reads, writes):
        for k in writes:
            self.last_w[k] = ev
            self.readers[k] = {}
        for k in reads:
            r = self.readers.setdefault(k, {})
            if r.get(ev[0], 0) < ev[1]:
                r[ev[0]] = ev[1]

    def op(self, eng, emit, reads=(), writes=()):
        waits = self._deps(eng, reads, writes)
        self.cnt[eng] += 1
        idx = self.cnt[eng]
        self.q[eng].append((waits, emit, True))
        self._commit((eng, idx), reads, writes)
        self.n_ops += 1

    def dma(self, qeng, out, in_, reads=(), writes=(), **kw):
        s = self.dma_rr
        self.dma_rr = (s + 1) % NDMA
        waits = self._deps(qeng, reads, writes)
        prev = self.dma_cnt[s] * 16
        key = ("dma", s)
        if prev > 0 and self.known[qeng].get(key, 0) < prev:
            waits.append((key, prev))
            self.known[qeng][key] = prev
        self.dma_cnt[s] += 1
        tgt = self.dma_cnt[s] * 16
        sem = self.sems[key]

        def emit(e, out=out, in_=in_, sem=sem, kw=kw):
            e.dma_start(out=out, in_=in_, **kw).then_inc(sem, 16)
            return None

        self.q[qeng].append((waits, emit, False))
        self._commit((key, tgt), reads, writes)
        self.n_ops += 1

    def barrier(self):
        evs = []
        for s in range(NDMA):
            if self.dma_cnt[s] > 0:
                evs.append((("dma", s), self.dma_cnt[s] * 16))
        for e in ("pe", "act", "dve", "pool"):
            if self.cnt[e] > 0:
                evs.append((e, self.cnt[e]))
        for e in self.ENG:
            waits = []
            for k, v in evs:
                if k == e and e == "pe":
                    continue
                if self.known[e].get(k, 0) < v:
                    waits.append((k, v))
                    self.known[e][k] = v
            if waits:
                self.q[e].append((waits, None, False))

    def finish(self):
        self.barrier()

    def build(self):
        nc = self.nc
        sems = self.sems
        q = self.q

        def replay(name, eng):
            mysem = sems[name]
            for waits, emit, inc in q[name]:
                for k, v in waits:
                    eng.wait_ge(sems[k], v)
                if emit is None:
                    continue
                ins = emit(eng)
                if inc:
                    ins.then_inc(mysem, 1)

        with nc.Block() as block:
            @block.tensor
            def _(e):
                replay("pe", e)

            @block.scalar
            def _(e):
                replay("act", e)

            @block.vector
            def _(e):
                replay("dve", e)

            @block.gpsimd
            def _(e):
                replay("pool", e)

            @block.sync
            def _(e):
                replay("sp", e)
        self.es.close()


class K:
    def __init__(self, P):
        self.P = P
        self.rr = {}

    def mm(self, out, lhsT, rhs, start, stop, reads, writes):
        self.P.op("pe", lambda e: e.matmul(out, lhsT=lhsT, rhs=rhs, start=start, stop=stop,
                                           skip_group_check=True), reads=reads, writes=writes)

    def tr(self, out, in_, ident, reads, writes):
        self.P.op("pe", lambda e: e.transpose(out, in_, ident), reads=reads, writes=writes)

    def act(self, out, in_, func, reads, writes, scale=1.0, bias=0.0, accum_out=None, eng="act"):
        if accum_out is None:
            self.P.op(eng, lambda e: e.activation(out=out, in_=in_, func=func, scale=scale, bias=bias),
                      reads=reads, writes=writes)
        else:
            self.P.op(eng, lambda e: e.activation(out=out, in_=in_, func=func, scale=scale, bias=bias,
                                                  accum_out=accum_out), reads=reads, writes=writes)

    def tt(self, eng, out, in0, in1, op, reads, writes):
        self.P.op(eng, lambda e: e.tensor_tensor(out=out, in0=in0, in1=in1, op=op), reads=reads, writes=writes)

    def ts(self, eng, out, in0, s1, op0, reads, writes, s2=None, op1=None, accum_out=None):
        if op1 is None:
            self.P.op(eng, lambda e: e.tensor_scalar(out=out, in0=in0, scalar1=s1, scalar2=None, op0=op0),
                      reads=reads, writes=writes)
        elif accum_out is None:
            self.P.op(eng, lambda e: e.tensor_scalar(out=out, in0=in0, scalar1=s1, scalar2=s2, op0=op0, op1=op1),
                      reads=reads, writes=writes)
        else:
            self.P.op(eng, lambda e: e.tensor_scalar(out=out, in0=in0, scalar1=s1, scalar2=s2, op0=op0, op1=op1,
                                                     accum_out=accum_out), reads=reads, writes=writes)

    def stt(self, out, in0, scalar, in1, op0, op1, reads, writes, accum_out=None):
        if accum_out is None:
            self.P.op("dve", lambda e: e.scalar_tensor_tensor(out=out, in0=in0, scalar=scalar, in1=in1,
                                                              op0=op0, op1=op1), reads=reads, writes=writes)
        else:
            self.P.op("dve", lambda e: e.scalar_tensor_tensor(out=out, in0=in0, scalar=scalar, in1=in1,
                                                              op0=op0, op1=op1, accum_out=accum_out),
                      reads=reads, writes=writes)

    def copy(self, eng, out, in_, reads, writes):
        if eng == "act":
            self.P.op("act", lambda e: e.activation(out=out, in_=in_, func=AF.Copy), reads=reads, writes=writes)
        else:
            self.P.op(eng, lambda e: e.tensor_copy(out=out, in_=in_), reads=reads, writes=writes)

    def memset(self, eng, ap, val, writes):
        self.P.op(eng, lambda e: e.memset(ap, val), writes=writes)

    def rot(self, name, n):
        i = self.rr.get(name, 0)
        self.rr[name] = (i + 1) % n
        return i


def bcast_rows(ap1d, n):
    return ap1d.unsqueeze(0).to_broadcast([128, n])


def layer_norm_tile(P, k, z, kz, gam, bet, eps, tmp):
    st, mv, rs = tmp["st"], tmp["mv"], tmp["rs"]
    kt = tmp["key"]
    for dh in range(2):
        P.op("dve", (lambda dh: lambda e: e.bn_stats(out=st[:, dh, :], in_=z[:, dh * 512:(dh + 1) * 512]))(dh),
             reads=[kz], writes=[kt])
    P.op("dve", lambda e: e.bn_aggr(out=mv[:], in_=st[:].rearrange("p a b -> p (a b)")), reads=[kt], writes=[kt])
    k.ts("dve", rs[:], mv[:, 1:2], eps, ALU.add, reads=[kt], writes=[kt])
    k.act(rs[:], rs[:], AF.Sqrt, reads=[kt], writes=[kt])
    P.op("dve", lambda e: e.reciprocal(out=rs[:], in_=rs[:]), reads=[kt], writes=[kt])
    k.ts("dve", z[:], z[:], mv[:, 0:1], ALU.subtract, reads=[kz, kt], writes=[kz], s2=rs[:, 0:1], op1=ALU.mult)
    k.tt("pool", z[:], z[:], gam[:], ALU.mult, reads=[kz, "lnp"], writes=[kz])
    k.tt("pool", z[:], z[:], bet[:], ALU.add, reads=[kz, "lnp"], writes=[kz])


def ffn_stage(P, k, C, src, ksrc, dst, kdst, wi_d, wo_d, g_d, b_d):
    P.stage_begin()
    ident = C["ident"]
    PW = C["PW"]
    ACC = C["ACC"]
    wi = P.sb([128, 8, 2 * DFF], BF16)
    wo = P.sb([128, 22, D], BF16)
    gam = P.sb([128, D], F32)
    bet = P.sb([128, D], F32)
    xt = [P.sb([128, D], F32) for _ in range(4)]
    xT = [P.sb([128, 8, 256], BF16) for _ in range(2)]
    sa = [P.sb([128, 256], F32) for _ in range(2)]
    hT = [P.sb([128, 256], BF16) for _ in range(3)]
    zt = [P.sb([128, D], F32) for _ in range(2)]
    st = P.sb([128, 2, 6], F32)
    mv = P.sb([128, 2], F32)
    rs = P.sb([128, 1], F32)
    tmp = {"st": st, "mv": mv, "rs": rs, "key": "lntmp"}

    P.dma("sp", gam[:], bcast_rows(g_d, D), writes=["lnp"])
    P.dma("sp", bet[:], bcast_rows(b_d, D), writes=["lnp"])
    for kc in range(8):
        for hf in range(2):
            P.dma("pool", wi[:, kc, hf * DFF:(hf + 1) * DFF], wi_d[kc * 128:(kc + 1) * 128, hf * DFF:(hf + 1) * DFF],
                  writes=[("wi", kc, hf)])
    for fc in range(22):
        P.dma("pool", wo[:, fc, :], wo_d[fc * 128:(fc + 1) * 128, :], writes=[("wo", fc)])
    wi_keys = [("wi", kc, hf) for kc in range(8) for hf in range(2)]

    NG = SEQ // 256
    pwc = [0]

    def next_pw():
        i = pwc[0] % 2
        pwc[0] += 1
        return PW[i], ("PW", i)

    for g in range(NG):
        r0 = g * 256
        xs = [xt[(2 * g + t) % 4] for t in range(2)]
        kxs = [("xt", (2 * g + t) % 4) for t in range(2)]
        for t in range(2):
            P.dma("sp", xs[t][:], src[r0 + t * 128:r0 + (t + 1) * 128, :], reads=[ksrc], writes=[kxs[t]])
        xTg = xT[g % 2]
        kxT = ("xT", g % 2)
        for t in range(2):
            for hf in range(2):
                pw, kpw = next_pw()
                for j in range(4):
                    kc = hf * 4 + j
                    k.tr(pw[:, j * 128:(j + 1) * 128], xs[t][:, kc * 128:(kc + 1) * 128], ident[:],
                         reads=[kxs[t], "ident"], writes=[kpw])
                k.copy("act", xTg[:, hf * 4:(hf + 1) * 4, t * 128:(t + 1) * 128],
                       pw[:].rearrange("p (a b) -> p a b", a=4), reads=[kpw], writes=[kxT])

        def h_mm(f):
            pw, kpw = next_pw()
            for hf in range(2):
                for kc in range(8):
                    k.mm(pw[:, hf * 256:(hf + 1) * 256], wi[:, kc, hf * DFF + f * 128: hf * DFF + (f + 1) * 128],
                         xTg[:, kc, :], kc == 0, kc == 7, reads=[kxT, ("wi", kc, hf)], writes=[kpw])
            s = sa[f % 2]
            h = hT[f % 3]
            k.act(s[:], pw[:, 0:256], AF.Silu, reads=[kpw], writes=[("sa", f % 2)])
            k.tt("dve", h[:], s[:], pw[:, 256:512], ALU.mult, reads=[("sa", f % 2), kpw], writes=[("hT", f % 3)])

        def o_mm(f):
            h = hT[f % 3]
            for t in range(2):
                for dh in range(2):
                    k.mm(ACC[t * 2 + dh][:], h[:, t * 128:(t + 1) * 128], wo[:, f, dh * 512:(dh + 1) * 512],
                         f == 0, f == 21, reads=[("hT", f % 3), ("wo", f)], writes=[("ACC", t * 2 + dh)])

        h_mm(0)
        for f in range(1, 22):
            h_mm(f)
            o_mm(f - 1)
        o_mm(21)

        for t in range(2):
            z = zt[t]
            kz = ("zt", t)
            for dh in range(2):
                k.stt(z[:, dh * 512:(dh + 1) * 512], xs[t][:, dh * 512:(dh + 1) * 512], 2.0 * ALPHA,
                      ACC[t * 2 + dh][:], ALU.mult, ALU.add, reads=[kxs[t], ("ACC", t * 2 + dh)], writes=[kz])
            layer_norm_tile(P, k, z, kz, gam, bet, 4.0 * LN_EPS, tmp)
            P.dma("sp", dst[r0 + t * 128:r0 + (t + 1) * 128, :], z[:], reads=[kz], writes=[kdst])


def load_transposed(P, k, C, xs, kxs, xTg, kxT, col0, ncols=128):
    ident = C["ident"]
    for hf in range(2):
        pw, kpw = C["bank"]()
        for j in range(4):
            kc = hf * 4 + j
            k.tr(pw[:, j * 128:(j + 1) * 128], xs[:, kc * 128:(kc + 1) * 128], ident[:],
                 reads=[kxs, "ident"], writes=[kpw])
        k.copy("act", xTg[:, hf * 4:(hf + 1) * 4, col0:col0 + 128],
               pw[:].rearrange("p (a b) -> p a b", a=4), reads=[kpw], writes=[kxT])


def mixb_stage(P, k, C, x1_d, kx1, o_d, ko, dst, kdst, win_d, wbr_d, wout_d, g_d, b_d):
    P.stage_begin()
    wg = P.sb([128, 8, 4096], BF16)
    wbr = P.sb([128, 8, D], BF16)
    wout = P.sb([128, 8, D], BF16)
    gam = P.sb([128, D], F32)
    bet = P.sb([128, D], F32)
    xt = [P.sb([128, D], F32) for _ in range(2)]
    ot = [P.sb([128, D], F32) for _ in range(2)]
    xT = [P.sb([128, 8, 128], BF16) for _ in range(2)]
    oT = [P.sb([128, 8, 128], BF16) for _ in range(2)]
    mT = [P.sb([128, 8, 128], BF16) for _ in range(2)]
    sg = [P.sb([128, 512], F32) for _ in range(2)]
    pr = [P.sb([128, 512], F32) for _ in range(2)]
    mg = [P.sb([128, D], F32) for _ in range(2)]
    zt = [P.sb([128, D], F32) for _ in range(2)]
    tmp = {"st": P.sb([128, 2, 6], F32), "mv": P.sb([128, 2], F32), "rs": P.sb([128, 1], F32), "key": "lntmp"}
    P.dma("sp", gam[:], bcast_rows(g_d, D), writes=["lnp"])
    P.dma("sp", bet[:], bcast_rows(b_d, D), writes=["lnp"])
    for kc in range(8):
        for q4 in range(4):
            P.dma("pool", wg[:, kc, q4 * 1024:(q4 + 1) * 1024],
                  win_d[kc * 128:(kc + 1) * 128, NSM + q4 * 1024: NSM + (q4 + 1) * 1024], writes=[("wg", kc)])
    for n in range(4):
        for c2 in range(2):
            P.dma("pool", wbr[:, n * 2 + c2, :], wbr_d[n, c2 * 128:(c2 + 1) * 128, :], writes=["wbr"])
    for kc in range(8):
        P.dma("pool", wout[:, kc, :], wout_d[kc * 128:(kc + 1) * 128, :], writes=["wout"])
    for t in range(SEQ // 128):
        r0 = t * 128
        b = t % 2
        P.dma("sp", xt[b][:], x1_d[r0:r0 + 128, :], reads=[kx1], writes=[("xt", b)])
        P.dma("sp", ot[b][:], o_d[r0:r0 + 128, :], reads=[ko], writes=[("ot", b)])
        load_transposed(P, k, C, xt[b], ("xt", b), xT[b], ("xT", b), 0)
        load_transposed(P, k, C, ot[b], ("ot", b), oT[b], ("oT", b), 0)
        m = mg[b]
        km = ("mg", b)
        for dh in range(2):
            for n in range(4):
                gp, kgp = C["bank"]()
                for kc in range(8):
                    k.mm(gp[:], xT[b][:, kc, :], wg[:, kc, n * 1024 + dh * 512: n * 1024 + (dh + 1) * 512],
                         kc == 0, kc == 7, reads=[("xT", b), ("wg", kc)], writes=[kgp])
                up, kup = C["bank"]()
                for c2 in range(2):
                    k.mm(up[:], oT[b][:, n * 2 + c2, :], wbr[:, n * 2 + c2, dh * 512:(dh + 1) * 512],
                         c2 == 0, c2 == 1, reads=[("oT", b), "wbr"], writes=[kup])
                si = k.rot("sg", 2)
                k.act(sg[si][:], gp[:], AF.Sigmoid, reads=[kgp], writes=[("sg", si)])
                if n == 0:
                    k.tt("dve", m[:, dh * 512:(dh + 1) * 512], sg[si][:], up[:], ALU.mult,
                         reads=[("sg", si), kup], writes=[km])
                else:
                    pi = k.rot("pr", 2)
                    k.tt("dve", pr[pi][:], sg[si][:], up[:], ALU.mult, reads=[("sg", si), kup], writes=[("pr", pi)])
                    k.tt("pool", m[:, dh * 512:(dh + 1) * 512], m[:, dh * 512:(dh + 1) * 512], pr[pi][:], ALU.add,
                         reads=[km, ("pr", pi)], writes=[km])
        load_transposed(P, k, C, m, km, mT[b], ("mT", b), 0)
        z = zt[b]
        kz = ("zt", b)
        for dh in range(2):
            ac, kac = C["bank"]()
            for kc in range(8):
                k.mm(ac[:], mT[b][:, kc, :], wout[:, kc, dh * 512:(dh + 1) * 512], kc == 0, kc == 7,
                     reads=[("mT", b), "wout"], writes=[kac])
            k.stt(z[:, dh * 512:(dh + 1) * 512], xt[b][:, dh * 512:(dh + 1) * 512], ALPHA, ac[:],
                  ALU.mult, ALU.add, reads=[("xt", b), kac], writes=[kz])
        layer_norm_tile(P, k, z, kz, gam, bet, LN_EPS, tmp)
        P.dma("sp", dst[r0:r0 + 128, :], z[:], reads=[kz], writes=[kdst])


NIT = 20
BIGM = 1.0e9
LNS = float(2 ** 30)
SLOPES = [2.0 ** (-2 * (h + 1)) for h in range(4)]


def load_w(P, dst, src2d, key):
    P.dma("pool", dst, src2d.rearrange("(kc p) f -> p kc f", p=128), writes=[key])


def mixa1_stage(P, k, C, x1_d, kx1, o_d, ko, win_d, CD):
    P.stage_begin()
    ident = C["ident"]
    wq = P.sb([128, 8, 256], BF16)
    wk = P.sb([128, 8, 64], BF16)
    wiq = P.sb([128, 8, 128], BF16)
    wik4 = P.sb([128, 8, 128], BF16)
    wtm = P.sb([128, 8, 68], BF16)
    load_w(P, wq[:], win_d[:, 0:256], "w_a1")
    load_w(P, wk[:], win_d[:, 256:320], "w_a1")
    load_w(P, wtm[:, :, 0:64], win_d[:, 320:384], "w_a1")
    load_w(P, wiq[:, :, 0:96], win_d[:, 384:480], "w_a1")
    wiqB = P.sb([128, 8, 32], BF16)
    load_w(P, wiqB[:], win_d[:, 480:512], "w_a1")
    IQTB = P.sb([128, 512], BF16)
    for r in range(4):
        load_w(P, wik4[:, :, r * 32:(r + 1) * 32], win_d[:, 512:544], "w_a1")
    load_w(P, wtm[:, :, 64:68], win_d[:, 544:548], "w_a1")
    identb = P.sb([128, 128], BF16)
    P.dma("pool", identb[:], CD["c_ident"], writes=["identb"])
    KT = P.sb([128, SEQ], BF16)
    IKT4 = P.sb([128, SEQ], BF16)
    V = P.sb([128, 32, 64], BF16)
    P.dma("pool", KT[64:66, :], CD["c_kpos"], writes=["KTpos"])
    QA = [P.sb([128, 512], BF16) for _ in range(4)]
    for h in range(4):
        P.dma("pool", QA[h][64:66, :], CD["c_qaug"][h], writes=[("QAc", h)])
    BQ = P.sb([128, 32, 4], F32)
    P.dma("sp", BQ[:], CD["c_bq"], writes=["BQ"])
    LCORR = P.sb([128, 4, 128], F32)
    P.dma("sp", LCORR[:], CD["c_corr"], writes=["CORR"])
    QPOS = P.sb([128, 32], F32)
    P.dma("sp", QPOS[:], CD["c_qpos"], writes=["QPOS"])
    NSL = P.sb([128, 4], F32)
    P.dma("sp", NSL[:], CD["c_nsl"], writes=["NSL"])
    B4 = P.sb([128, 4], F32)
    PW2 = P.sb([128, 2, NIT], F32)
    P.dma("sp", PW2[:], CD["c_pw2"], writes=["PW2"])
    IQT = P.sb([128, 512], BF16)
    IW = [P.sb([128, 4], F32) for _ in range(4)]
    xt = [P.sb([128, D], F32) for _ in range(2)]
    xT = P.sb([128, 8, 512], BF16)
    isc = P.sb([128, SEQ], F32)
    kf = P.sb([128, SEQ], F32)
    NEGIDX = P.sb([128, SEQ], F32)
    P.dma("sp", NEGIDX[:], CD["c_negidx"], writes=["NEGIDX"])
    KPOSF = P.sb([128, SEQ], F32)
    k.ts("pool", KPOSF[:], NEGIDX[:], -4096.0, ALU.mult, reads=["NEGIDX"], writes=["KPOSF"])
    tmpk = P.sb([128, SEQ], F32)
    lgs = [P.sb([128, 512], F32) for _ in range(2)]
    msk = P.sb([128, SEQ], BF16)
    MD = P.sb([128, 4, 128], F32)
    rl = [P.sb([128, 512], F32) for _ in range(2)]
    Eb = [P.sb([128, 512], BF16) for _ in range(2)]
    Pm = [P.sb([128, 512], BF16) for _ in range(2)]
    PT = [P.sb([128, 512], BF16) for _ in range(2)]
    RS = P.sb([128, 16], F32)
    sm = P.sb([128, 16], F32)
    W2 = P.sb([128, 2, NIT], F32)
    OA = [P.sb([128, 256], F32) for _ in range(2)]
    accb = C["banks"][0:2]
    wb = C["wbank"]
    bb = C["bbank"]

    for G in range(SEQ // 512):
        c0 = G * 512
        for tt in range(4):
            b = k.rot("a1xt", 2)
            P.dma("sp", xt[b][:], x1_d[c0 + tt * 128:c0 + (tt + 1) * 128, :], reads=[kx1], writes=[("xt", b)])
            load_transposed(P, k, C, xt[b], ("xt", b), xT, "xT", tt * 128)
        for h in range(4):
            pb, kpb = wb()
            for kc in range(8):
                k.mm(pb[0:64, :], wq[:, kc, 64 * h:64 * h + 64], xT[:, kc, :], kc == 0, kc == 7,
                     reads=["xT", "w_a1"], writes=[kpb])
            k.copy("act" if h % 2 == 0 else "dve", QA[h][0:64, :], pb[0:64, :], reads=[kpb], writes=[("QA", h)])
        pb, kpb = wb()
        for kc in range(8):
            k.mm(pb[0:64, :], wk[:, kc, :], xT[:, kc, :], kc == 0, kc == 7, reads=["xT", "w_a1"], writes=[kpb])
        k.copy("dve", KT[0:64, c0:c0 + 512], pb[0:64, :], reads=[kpb], writes=[("KT", G)])
        pb, kpb = wb()
        for kc in range(8):
            k.mm(pb[:], wik4[:, kc, :], xT[:, kc, :], kc == 0, kc == 7, reads=["xT", "w_a1"], writes=[kpb])
        k.copy("act", IKT4[:, c0:c0 + 512], pb[:], reads=[kpb], writes=[("IKT", G)])
        pb, kpb = wb()
        for kc in range(8):
            k.mm(pb[0:96, :], wiq[:, kc, 0:96], xT[:, kc, :], kc == 0, kc == 7, reads=["xT", "w_a1"], writes=[kpb])
        k.copy("dve", IQT[0:96, :], pb[0:96, :], reads=[kpb], writes=["IQT"])
        pb, kpb = wb()
        for kc in range(8):
            k.mm(pb[0:32, :], wiqB[:, kc, :], xT[:, kc, :], kc == 0, kc == 7, reads=["xT", "w_a1"], writes=[kpb])
        k.copy("act", IQTB[0:32, :], pb[0:32, :], reads=[kpb], writes=["IQT"])
        for tt in range(4):
            T = 4 * G + tt
            pb, kpb = wb()
            for kc in range(8):
                k.mm(pb[:, 0:68], xT[:, kc, tt * 128:(tt + 1) * 128], wtm[:, kc, :], kc == 0, kc == 7,
                     reads=["xT", "w_a1"], writes=[kpb])
            k.copy("act", V[:, T, :], pb[:, 0:64], reads=[kpb], writes=[("V", G)])
            k.copy("dve", IW[tt][:], pb[:, 64:68], reads=[kpb], writes=[("IW", tt)])

        for tt in range(4):
            T = 4 * G + tt
            nk = 128 * (T + 1)
            NKT = (nk + 511) // 512
            kread = [("KT", g) for g in range(G + 1)] + ["KTpos"]
            ikread = [("IKT", g) for g in range(G + 1)]
            vread = [("V", g) for g in range(G + 1)]
            qs = slice(tt * 128, (tt + 1) * 128)
            for kt in range(NKT):
                wk_ = min(512, nk - 512 * kt)
                ks_ = slice(kt * 512, kt * 512 + wk_)
                for h in range(4):
                    z, kz = wb()
                    if h < 3:
                        k.mm(z[:, 0:wk_], IQT[32 * h:32 * h + 32, qs], IKT4[32 * h:32 * h + 32, ks_], True, True,
                             reads=["IQT"] + ikread, writes=[kz])
                    else:
                        k.mm(z[:, 0:wk_], IQTB[0:32, qs], IKT4[0:32, ks_], True, True,
                             reads=["IQT"] + ikread, writes=[kz])
                    ri = k.rot("rl", 2)
                    k.act(rl[ri][:, 0:wk_], z[:, 0:wk_], AF.Relu, reads=[kz], writes=[("rl", ri)])
                    if h == 0:
                        k.ts("dve", isc[:, ks_], rl[ri][:, 0:wk_], IW[tt][:, 0:1], ALU.mult,
                             reads=[("rl", ri), ("IW", tt)], writes=["isc"])
                    else:
                        k.stt(isc[:, ks_], rl[ri][:, 0:wk_], IW[tt][:, h:h + 1], isc[:, ks_], ALU.mult, ALU.add,
                              reads=[("rl", ri), ("IW", tt), "isc"], writes=["isc"])
            k.memset("pool", isc[0:64, nk - 64:nk], -BIGM, writes=["isc"])
            k.act(kf[:, 0:nk], isc[:, 0:nk], AF.Abs, reads=["isc"], writes=["kf"])
            k.act(kf[:, 0:nk], kf[:, 0:nk], AF.Ln, reads=["kf"], writes=["kf"], scale=LNS, bias=1.0)
            k.act(isc[:, 0:nk], isc[:, 0:nk], AF.Sign, reads=["isc"], writes=["isc"])
            k.tt("pool", kf[:, 0:nk], kf[:, 0:nk], isc[:, 0:nk], ALU.mult, reads=["kf", "isc"], writes=["kf"])
            k.stt(isc[:, 0:nk], kf[:, 0:nk], 0.0, NEGIDX[:, 0:nk], ALU.is_equal, ALU.mult,
                  reads=["kf", "NEGIDX", "isc"], writes=["isc"])
            k.tt("pool", kf[:, 0:nk], kf[:, 0:nk], isc[:, 0:nk], ALU.add, reads=["kf", "isc"], writes=["kf"])
            if T <= 1:
                k.memset("dve", sm[:, 6:7], -40.0, writes=["sm"])
            else:
                P.op("dve", (lambda nk: lambda e: e.reduce_max(out=sm[:, 0:1], in_=kf[:, 0:nk], axis=AX.X))(nk),
                     reads=["kf"], writes=["sm"])
                P.op("dve", (lambda nk: lambda e: e.tensor_reduce(out=sm[:, 1:2], in_=kf[:, 0:nk - 64], axis=AX.X,
                                                                   op=ALU.min))(nk), reads=["kf"], writes=["sm"])
                k.ts("dve", sm[:, 1:2], sm[:, 1:2], -1.0, ALU.add, reads=["sm"], writes=["sm"])
                k.tt("dve", sm[:, 2:3], sm[:, 0:1], sm[:, 1:2], ALU.subtract, reads=["sm"], writes=["sm"])
                k.ts("dve", W2[:].rearrange("p a b -> p (a b)"), PW2[:].rearrange("p a b -> p (a b)"), sm[:, 2:3],
                     ALU.mult, reads=["sm", "PW2"], writes=["W2"])
                k.stt(sm[:, 3:4], sm[:, 2:3], 0.5, sm[:, 1:2], ALU.mult, ALU.add, reads=["sm"], writes=["sm"])
                for it in range(NIT):
                    k.ts("dve", msk[:, 0:nk], kf[:, 0:nk], sm[:, 3:4], ALU.is_gt, reads=["kf", "sm"],
                         writes=["msk", "sm"], s2=None, op1=ALU.add, accum_out=sm[:, 4:5])
                    k.ts("dve", sm[:, 5:6], sm[:, 4:5], 255.5, ALU.is_gt, reads=["sm", "W2"], writes=["sm"],
                         s2=W2[:, 0, it:it + 1], op1=ALU.mult)
                    k.stt(sm[:, 3:4], sm[:, 5:6], W2[:, 1, it:it + 1], sm[:, 3:4], ALU.add, ALU.add,
                          reads=["sm", "W2"], writes=["sm"])
                k.tt("dve", sm[:, 6:7], sm[:, 3:4], W2[:, 1, NIT - 1:NIT], ALU.add, reads=["sm", "W2"], writes=["sm"])
            k.ts("dve", isc[:, 0:nk], kf[:, 0:nk], sm[:, 6:7], ALU.is_le, reads=["kf", "sm", "isc"], writes=["isc"],
                 s2=-30000.0, op1=ALU.mult)
            k.tt("pool", tmpk[:, 0:nk], KPOSF[:, 0:nk], isc[:, 0:nk], ALU.add, reads=["KPOSF", "isc"], writes=["tmpk"])
            P.op("dve", (lambda nk: lambda e: e.reduce_max(out=sm[:, 9:10], in_=tmpk[:, 0:nk], axis=AX.X))(nk),
                 reads=["tmpk"], writes=["sm"])
            k.ts("dve", sm[:, 9:10], sm[:, 9:10], QPOS[:, T:T + 1], ALU.min, reads=["sm", "QPOS"], writes=["sm"])
            k.ts("dve", B4[:], NSL[:], sm[:, 9:10], ALU.mult, reads=["sm", "NSL"], writes=["B4"])
            for h in range(4):
                k.tt("pool", MD[:, h, :], isc[:, nk - 128:nk], LCORR[:, h, :], ALU.add, reads=["isc", "CORR"],
                     writes=["MD"])
            oa = OA[T % 2]
            koa = ("OA", T % 2)
            for h in range(4):
                acc = accb[h % 2]
                kacc = ("ACC", h % 2)
                nslot = 0
                for kt in range(NKT):
                    wk_ = min(512, nk - 512 * kt)
                    ks_ = slice(kt * 512, kt * 512 + wk_)
                    lg, klg = wb()
                    k.mm(lg[:, 0:wk_], QA[h][0:66, qs], KT[0:66, ks_], True, True,
                         reads=[("QA", h), ("QAc", h)] + kread, writes=[klg])
                    last = kt == NKT - 1
                    wpl = wk_ - 128 if last else wk_
                    li = k.rot("lgs", 2)
                    if wpl > 0:
                        k.stt(lgs[li][:, 0:wpl], lg[:, 0:wpl], 0.125, isc[:, kt * 512:kt * 512 + wpl], ALU.mult,
                              ALU.add, reads=[klg, "isc"], writes=[("lgs", li)])
                    if last:
                        k.stt(lgs[li][:, wpl:wk_], lg[:, wpl:wk_], 0.125, MD[:, h, :], ALU.mult, ALU.add,
                              reads=[klg, "MD"], writes=[("lgs", li)])
                    pi = k.rot("Pm", 2)
                    k.act(Pm[pi][:, 0:wk_], lgs[li][:, 0:wk_], AF.Exp, reads=[("lgs", li), "B4"],
                          writes=[("Pm", pi), "RS"], bias=B4[:, h:h + 1], accum_out=RS[:, nslot:nslot + 1])
                    nslot += 1
                    nb = wk_ // 128
                    tp, ktp = bb()
                    for j in range(nb):
                        k.tr(tp[:, j * 128:(j + 1) * 128], Pm[pi][:, j * 128:(j + 1) * 128], identb[:],
                             reads=[("Pm", pi), "identb"], writes=[ktp])
                    ti = k.rot("PT", 2)
                    k.copy("act" if kt % 2 == 0 else "dve", PT[ti][:, 0:wk_], tp[:, 0:wk_], reads=[ktp],
                           writes=[("PT", ti)])
                    for j in range(nb):
                        k.mm(acc[:, 0:64], PT[ti][:, j * 128:(j + 1) * 128], V[:, kt * 4 + j, :],
                             kt == 0 and j == 0, last and j == nb - 1, reads=[("PT", ti)] + vread, writes=[kacc])
                P.op("dve", (lambda n: lambda e: e.reduce_sum(out=sm[:, 7:8], in_=RS[:, 0:n], axis=AX.X))(nslot),
                     reads=["RS"], writes=["sm"])
                P.op("dve", lambda e: e.reciprocal(out=sm[:, 8:9], in_=sm[:, 7:8]), reads=["sm"], writes=["sm"])
                k.ts("dve", oa[:, 64 * h:64 * h + 64], acc[:, 0:64], sm[:, 8:9], ALU.mult, reads=[kacc, "sm"],
                     writes=[koa])
            P.dma("sp", o_d[T * 128:(T + 1) * 128, 0:256], oa[:], reads=[koa], writes=[ko])


def small_rstd(P, k, ss, key, scale, eps):
    k.ts("dve", ss, ss, scale, ALU.mult, reads=[key], writes=[key], s2=eps, op1=ALU.add)
    k.act(ss, ss, AF.Sqrt, reads=[key], writes=[key])
    P.op("dve", lambda e: e.reciprocal(out=ss, in_=ss), reads=[key], writes=[key])


def rope_ops(P, k, x1, x2, cosb, sinb, o1, o2, tmps, kin, kout, ktmp):
    t1, t2, t3, t4 = tmps
    k.tt("dve", t1, cosb, x1, ALU.mult, reads=[kin, "rope"], writes=[ktmp])
    k.tt("dve", t2, sinb, x2, ALU.mult, reads=[kin, "rope"], writes=[ktmp])
    k.tt("pool", o1, t1, t2, ALU.subtract, reads=[ktmp], writes=[kout])
    k.tt("dve", t3, sinb, x1, ALU.mult, reads=[kin, "rope"], writes=[ktmp])
    k.tt("dve", t4, cosb, x2, ALU.mult, reads=[kin, "rope"], writes=[ktmp])
    k.tt("pool", o2, t3, t4, ALU.add, reads=[ktmp], writes=[kout])


def mixa2_stage(P, k, C, x1_d, kx1, o_d, ko, win_d, CD, gn_d, qn_d, kvn_d, wuq_d, wukv_d, do_ret=True, do_mla=True):
    P.stage_begin()
    banks = C["banks"]
    wc2 = [0]

    def wb():
        i = 4 + wc2[0] % 2
        wc2[0] += 1
        return banks[i], ("PW", i - 4)
    bb = C["bbank"]
    wrq = P.sb([128, 8, 256], BF16)
    wrk = P.sb([128, 8, 256], BF16)
    wtm = P.sb([128, 8, 1184], BF16)
    load_w(P, wrq[:], win_d[:, 548:804], "w_a2")
    load_w(P, wrk[:], win_d[:, 804:1060], "w_a2")
    load_w(P, wtm[:, :, 0:768], win_d[:, 804:1572], "w_a2")
    load_w(P, wtm[:, :, 768:1184], win_d[:, 1828:2244], "w_a2")
    identb = P.sb([128, 128], BF16)
    P.dma("pool", identb[:], CD["c_ident"], writes=["identb"])
    wuq_f = P.sb([128, 2, 384], F32)
    wukv_f = P.sb([128, 512], F32)
    qn = P.sb([128, 2], F32)
    kvn = P.sb([128, 1], F32)
    wuq = P.sb([128, 2, 384], BF16)
    wukv = P.sb([128, 512], BF16)
    P.dma("sp", wuq_f[:], wuq_d.rearrange("(c p) f -> p c f", p=128), writes=["wuq_f"])
    P.dma("sp", wukv_f[:], wukv_d, writes=["wukv_f"])
    for c2 in range(2):
        P.dma("sp", qn[:, c2:c2 + 1], qn_d[c2 * 128:(c2 + 1) * 128].rearrange("(p c) -> p c", c=1), writes=["qn"])
    P.dma("sp", kvn[:], kvn_d.rearrange("(p c) -> p c", c=1), writes=["kvn"])
    for c2 in range(2):
        k.ts("dve", wuq[:, c2, :], wuq_f[:, c2, :], qn[:, c2:c2 + 1], ALU.mult, reads=["wuq_f", "qn"], writes=["wuq"])
    k.ts("dve", wukv[:], wukv_f[:], kvn[:, 0:1], ALU.mult, reads=["wukv_f", "kvn"], writes=["wukv"])
    XI = P.sb([128, 2, 512], F32)
    P.dma("sp", XI[:], CD["c_xi"], writes=["XI"])
    ZETA = P.sb([128, 256], F32)
    P.dma("sp", ZETA[:], CD["c_zeta"], writes=["ZETA"])
    DECT = P.sb([128, 512], F32)
    P.dma("sp", DECT[:], CD["c_dect"], writes=["DECT"])
    GCH = P.sb([128, 128], F32)
    P.dma("sp", GCH[:], CD["c_gch"], writes=["GCH"])
    CMT = P.sb([128, 512], BF16)
    P.dma("pool", CMT[:], CD["c_cmt"], writes=["CMT"])
    COS = P.sb([128, 32, 16], F32)
    SIN = P.sb([128, 32, 16], F32)
    P.dma("sp", COS[:], CD["c_cos"], writes=["rope"])
    P.dma("sp", SIN[:], CD["c_sin"], writes=["rope"])
    GNW = P.sb([128, 256], F32)
    P.dma("sp", GNW[:], bcast_rows(gn_d, 256), writes=["GNW"])
    KTm = P.sb([128, 4, SEQ], BF16)
    Vm = P.sb([128, 32, 4, 65], BF16)
    k.memset("pool", Vm[:].rearrange("p a b c -> p (a b c)"), 1.0, writes=["Vm1"])
    state = P.sb([128, 128], F32)
    state_b = P.sb([128, 128], BF16)
    k.memset("dve", state[:], 0.0, writes=["state"])
    k.memset("dve", state_b[:], 0.0, writes=["state_b"])
    xt = [P.sb([128, D], F32) for _ in range(2)]
    xT = P.sb([128, 8, 512], BF16)
    rqT = P.sb([128, 2, 512], BF16)
    rqxTz = [P.sb([128, 512], BF16) for _ in range(4)]
    rkTz = [P.sb([128, 512], BF16) for _ in range(4)]
    for h in range(4):
        k.memset("pool", rqxTz[h][:], 0.0, writes=["rqxTz"])
        k.memset("pool", rkTz[h][:], 0.0, writes=["rkTz"])
    rkz = [P.sb([128, 256], BF16) for _ in range(4)]
    rv = [P.sb([128, 256], BF16) for _ in range(4)]
    rgs = [P.sb([128, 256], F32) for _ in range(4)]
    cqn = [P.sb([128, 256], BF16) for _ in range(4)]
    ckvn = [P.sb([128, 128], BF16) for _ in range(4)]
    krs = [P.sb([128, 32], F32) for _ in range(4)]
    junk = P.sb([128, 256], F32)
    cqf = P.sb([128, 256], F32)
    qf = P.sb([128, 384], F32)
    kvf = P.sb([128, 512], F32)
    accs = P.sb([128, 4, 65], F32)
    ss = P.sb([128, 8], F32)
    PTr = P.sb([128, 512], BF16)
    st6 = P.sb([128, 4, 6], F32)
    mv4 = P.sb([128, 4, 2], F32)
    rs4 = P.sb([128, 4], F32)
    retn = P.sb([128, 256], F32)
    cqnT = P.sb([128, 2, 128], BF16)
    qtm = P.sb([128, 384], BF16)
    qT = P.sb([128, 4, 128], BF16)
    ckvnT = P.sb([128, 128], BF16)
    ktm = P.sb([128, 4, 96], BF16)
    krr = P.sb([128, 32], F32)
    rt = [P.sb([128, 64], F32) for _ in range(4)]
    PTm = [P.sb([128, 512], BF16) for _ in range(2)]
    od = [P.sb([128, 768], F32) for _ in range(2)]
    k.memset("pool", od[0][:], 0.0, writes=[("od", 0)])
    k.memset("pool", od[1][:], 0.0, writes=[("od", 1)])
    SC = 96.0 ** -0.5
    import os
    NGDBG = int(os.environ.get("A2_MAXG", SEQ // 512))
    if os.environ.get("A2_VAR", "") == "B":
        P.barrier()
    CUT = int(os.environ.get("A2_CUT", 9))
    CUTP = int(os.environ.get("A2_CUTP", 9))

    for G in range(NGDBG):
        c0 = G * 512
        for tt in range(4):
            b = k.rot("a2xt", 2)
            P.dma("sp", xt[b][:], x1_d[c0 + tt * 128:c0 + (tt + 1) * 128, :], reads=[kx1], writes=[("xt", b)])
            load_transposed(P, k, C, xt[b], ("xt", b), xT, "xT", tt * 128)
        if do_ret and CUTP >= 1:
            for t in range(2):
                pb, kpb = wb()
                for kc in range(8):
                    k.mm(pb[:], wrq[:, kc, t * 128:(t + 1) * 128], xT[:, kc, :], kc == 0, kc == 7,
                         reads=["xT", "w_a2"], writes=[kpb])
                k.copy("act", rqT[:, t, :], pb[:], reads=[kpb], writes=["rqT"])
                for hh in range(2 if CUTP >= 2 else 0):
                    rs_ = slice(64 * hh, 64 * hh + 64)
                    k.tt("dve", rqxTz[2 * t + hh][rs_, :], XI[rs_, t, :], pb[rs_, :], ALU.mult, reads=[kpb, "XI"],
                         writes=["rqxTz"])
                if CUTP < 3:
                    continue
                pb, kpb = wb()
                for kc in range(8):
                    k.mm(pb[:], wrk[:, kc, t * 128:(t + 1) * 128], xT[:, kc, :], kc == 0, kc == 7,
                         reads=["xT", "w_a2"], writes=[kpb])
                for hh in range(2):
                    rs_ = slice(64 * hh, 64 * hh + 64)
                    k.copy("act" if hh == 0 else "dve", rkTz[2 * t + hh][rs_, :], pb[rs_, :], reads=[kpb], writes=["rkTz"])
        for tt in range(4):
            ts_ = slice(tt * 128, (tt + 1) * 128)
            if do_ret:
                pb, kpb = wb()
                for kc in range(8):
                    k.mm(pb[:], xT[:, kc, ts_], wtm[:, kc, 0:512], kc == 0, kc == 7, reads=["xT", "w_a2"], writes=[kpb])
                if os.environ.get("A2_VAR", "") == "A":
                    k.copy("act", rkz[tt][:], pb[:, 0:256], reads=[kpb], writes=[("rkz", tt)])
                else:
                    k.tt("dve", rkz[tt][:], ZETA[:], pb[:, 0:256], ALU.mult, reads=[kpb, "ZETA"], writes=[("rkz", tt)])
                k.copy("act", rv[tt][:], pb[:, 256:512], reads=[kpb], writes=[("rv", tt)])
            pb, kpb = wb()
            for kc in range(8):
                k.mm(pb[:], xT[:, kc, ts_], wtm[:, kc, 512:1024], kc == 0, kc == 7, reads=["xT", "w_a2"], writes=[kpb])
            k.act(rgs[tt][:], pb[:, 0:256], AF.Silu, reads=[kpb], writes=[("rgs", tt)])
            if do_mla:
                k.act(junk[:], pb[:, 256:512], AF.Square, reads=[kpb], writes=["junk", "ss"], accum_out=ss[:, 0:1])
                small_rstd(P, k, ss[:, 0:1], "ss", 1.0 / 256.0, RMS_EPS)
                k.copy("act", cqf[:], pb[:, 256:512], reads=[kpb], writes=["cqf"])
                k.ts("dve", cqn[tt][:], cqf[:], ss[:, 0:1], ALU.mult, reads=["cqf", "ss"], writes=[("cqn", tt)])
                pb, kpb = wb()
                for kc in range(8):
                    k.mm(pb[:, 0:160], xT[:, kc, ts_], wtm[:, kc, 1024:1184], kc == 0, kc == 7,
                         reads=["xT", "w_a2"], writes=[kpb])
                k.act(junk[:, 0:128], pb[:, 0:128], AF.Square, reads=[kpb], writes=["junk", "ss"], accum_out=ss[:, 1:2])
                small_rstd(P, k, ss[:, 1:2], "ss", 1.0 / 128.0, RMS_EPS)
                k.copy("act", cqf[:, 0:128], pb[:, 0:128], reads=[kpb], writes=["cqf"])
                k.ts("dve", ckvn[tt][:], cqf[:, 0:128], ss[:, 1:2], ALU.mult, reads=["cqf", "ss"], writes=[("ckvn", tt)])
                k.copy("act", krs[tt][:], pb[:, 128:160], reads=[kpb], writes=[("krs", tt)])

        for tt in range(4):
            T = 4 * G + tt
            q0 = T * 128
            o = od[T % 2]
            kod = ("od", T % 2)
            if do_ret and CUT >= 1:
                O = banks[2]
                kO = ("ACC", 2)
                KV = banks[3]
                kKV = ("ACC", 3)
                ns_ = slice(tt * 128, (tt + 1) * 128)
                sT, ksT = wb()
                for h in range(4):
                    k.mm(sT[:, 128 * h:128 * h + 128], rkTz[h][:, ns_], rqT[:, h // 2, ns_], True, True,
                         reads=["rkTz", "rqT"], writes=[ksT])
                k.tt("dve", PTr[:], DECT[:], sT[:], ALU.mult, reads=[ksT, "DECT"], writes=["PTr"])
                for h in range(4 if CUT >= 2 else 0):
                    t = h // 2
                    k.mm(O[:, 64 * h:64 * h + 64], PTr[:, 128 * h:128 * h + 128], rv[tt][:, 64 * h:64 * h + 64],
                         True, False, reads=["PTr", ("rv", tt)], writes=[kO])
                    k.mm(O[:, 64 * h:64 * h + 64], rqxTz[h][:, ns_], state_b[:, 64 * t:64 * t + 64],
                         False, True, reads=["rqxTz", "state_b"], writes=[kO])
                for h in range(4 if CUT >= 3 else 0):
                    t = h // 2
                    k.mm(KV[:, 64 * h:64 * h + 64], rkz[tt][:, 128 * t:128 * t + 128], rv[tt][:, 64 * h:64 * h + 64],
                         True, True, reads=[("rkz", tt), ("rv", tt)], writes=[kKV])
                k.tt("pool", state[:], state[:], GCH[:], ALU.mult, reads=["state", "GCH"], writes=["state"])
                for h in range(4 if CUT >= 3 else 0):
                    hb = 64 * (h % 2)
                    t = h // 2
                    k.tt("dve", state[hb:hb + 64, 64 * t:64 * t + 64], state[hb:hb + 64, 64 * t:64 * t + 64],
                         KV[hb:hb + 64, 64 * h:64 * h + 64], ALU.add, reads=["state", kKV], writes=["state"])
                k.copy("act", state_b[:], state[:], reads=["state"], writes=["state_b"])
                for h in range(4 if CUT >= 4 else 0):
                    P.op("dve", (lambda h: lambda e: e.bn_stats(out=st6[:, h, :], in_=O[:, 64 * h:64 * h + 64]))(h),
                         reads=[kO], writes=["st6"])
                    P.op("dve", (lambda h: lambda e: e.bn_aggr(out=mv4[:, h, :], in_=st6[:, h, :]))(h),
                         reads=["st6"], writes=["mv4"])
                if CUT >= 4:
                    k.ts("dve", rs4[:], mv4[:, :, 1], LN_EPS, ALU.add, reads=["mv4"], writes=["rs4"])
                    k.act(rs4[:], rs4[:], AF.Sqrt, reads=["rs4"], writes=["rs4"])
                    P.op("dve", lambda e: e.reciprocal(out=rs4[:], in_=rs4[:]), reads=["rs4"], writes=["rs4"])
                for h in range(4 if CUT >= 4 else 0):
                    k.ts("dve", retn[:, 64 * h:64 * h + 64], O[:, 64 * h:64 * h + 64], mv4[:, h, 0:1], ALU.subtract,
                         reads=[kO, "mv4", "rs4"], writes=["retn"], s2=rs4[:, h:h + 1], op1=ALU.mult)
                k.tt("pool", retn[:], retn[:], GNW[:], ALU.mult, reads=["retn", "GNW"], writes=["retn"])
                k.tt("pool", o[:, 0:256], retn[:], rgs[tt][:], ALU.mult, reads=["retn", ("rgs", tt)], writes=[kod])
            if do_mla:
                tp, ktp = bb()
                for c2 in range(2):
                    k.tr(tp[:, c2 * 128:(c2 + 1) * 128], cqn[tt][:, c2 * 128:(c2 + 1) * 128], identb[:],
                         reads=[("cqn", tt), "identb"], writes=[ktp])
                k.copy("act", cqnT[:].rearrange("p a b -> p (a b)"), tp[:, 0:256], reads=[ktp], writes=["cqnT"])
                qp, kqp = wb()
                for c2 in range(2):
                    k.mm(qp[:, 0:384], cqnT[:, c2, :], wuq[:, c2, :], c2 == 0, c2 == 1, reads=["cqnT", "wuq"], writes=[kqp])
                k.copy("act", qf[:], qp[:, 0:384], reads=[kqp], writes=["qf"])
                kqp = "qf"
                qv = qf[:].rearrange("p (h c) -> p h c", c=96)
                qo = qtm[:].rearrange("p (h c) -> p h c", c=96)
                k.copy("pool", qo[:, :, 0:64], qv[:, :, 0:64], reads=[kqp], writes=["qtm"])
                cosb = COS[:, T:T + 1, :].to_broadcast([128, 4, 16])
                sinb = SIN[:, T:T + 1, :].to_broadcast([128, 4, 16])
                tm = [r_[:].rearrange("p (h c) -> p h c", c=16) for r_ in rt]
                rope_ops(P, k, qv[:, :, 64:80], qv[:, :, 80:96], cosb, sinb, qo[:, :, 64:80], qo[:, :, 80:96], tm,
                         kqp, "qtm", "rt")
                tp, ktp = bb()
                for h in range(4):
                    k.tr(tp[0:96, h * 128:(h + 1) * 128], qtm[:, 96 * h:96 * h + 96], identb[:],
                         reads=["qtm", "identb"], writes=[ktp])
                k.copy("dve", qT[0:96].rearrange("p a b -> p (a b)"), tp[0:96, 0:512], reads=[ktp], writes=["qT"])
                tp, ktp = bb()
                k.tr(tp[:, 0:128], ckvn[tt][:], identb[:], reads=[("ckvn", tt), "identb"], writes=[ktp])
                k.copy("act", ckvnT[:], tp[:, 0:128], reads=[ktp], writes=["ckvnT"])
                kp, kkp = wb()
                k.mm(kp[:], ckvnT[:], wukv[:], True, True, reads=["ckvnT", "wukv"], writes=[kkp])
                k.copy("act", kvf[:], kp[:], reads=[kkp], writes=["kvf"])
                kkp = "kvf"
                kvv = kvf[:].rearrange("p (h c) -> p h c", c=128)
                k.copy("pool", Vm[:, T, :, 0:64], kvv[:, :, 64:128], reads=[kkp, "Vm1"], writes=[("Vm", G)])
                k.copy("dve", ktm[:, :, 0:64], kvv[:, :, 0:64], reads=[kkp], writes=["ktm"])
                c1 = COS[:, T, :]
                s1 = SIN[:, T, :]
                rope_ops(P, k, krs[tt][:, 0:16], krs[tt][:, 16:32], c1, s1, krr[:, 0:16], krr[:, 16:32],
                         [r_[:, 0:16] for r_ in rt], ("krs", tt), "krr", "rt")
                k.copy("pool", ktm[:, :, 64:96], krr[:].unsqueeze(1).to_broadcast([128, 4, 32]), reads=["krr"],
                       writes=["ktm"])
                tp, ktp = bb()
                for h in range(4):
                    k.tr(tp[0:96, h * 128:(h + 1) * 128], ktm[:, h, :], identb[:], reads=["ktm", "identb"], writes=[ktp])
                k.copy("dve", KTm[0:96, :, q0:q0 + 128], tp[0:96, 0:512].rearrange("p (h c) -> p h c", c=128),
                       reads=[ktp], writes=[("KTm", G)])
                ktread = [("KTm", g) for g in range(G + 1)]
                vmread = [("Vm", g) for g in range(G + 1)] + ["Vm1"]
                for j in range(T + 1):
                    stp, kst = wb()
                    for h in range(4):
                        k.mm(stp[:, h * 128:(h + 1) * 128], KTm[0:96, h, j * 128:(j + 1) * 128], qT[0:96, h, :], True, True,
                             reads=["qT"] + ktread, writes=[kst])
                    pi = k.rot("PTm", 2)
                    k.act(PTm[pi][:], stp[:], AF.Exp, reads=[kst], writes=[("PTm", pi)], scale=SC)
                    if j == T:
                        k.tt("pool", PTm[pi][:], PTm[pi][:], CMT[:], ALU.mult, reads=[("PTm", pi), "CMT"],
                             writes=[("PTm", pi)])
                    for h in range(4):
                        k.mm(banks[h][:, 0:65], PTm[pi][:, h * 128:(h + 1) * 128], Vm[:, j, h, :], j == 0, j == T,
                             reads=[("PTm", pi)] + vmread, writes=[("ACC", h)])
                for h in range(4):
                    k.copy("act", accs[:, h, :], banks[h][:, 0:65], reads=[("ACC", h)], writes=["accs"])
                for h in range(4):
                    P.op("dve", (lambda h: lambda e: e.reciprocal(out=ss[:, 4 + h:5 + h], in_=accs[:, h, 64:65]))(h),
                         reads=["accs"], writes=["ss"])
                    k.ts("dve", o[:, 512 + 64 * h:512 + 64 * h + 64], accs[:, h, 0:64], ss[:, 4 + h:5 + h], ALU.mult,
                         reads=["accs", "ss"], writes=[kod])
            P.dma("sp", o_d[q0:q0 + 128, 256:512], o[:, 0:256], reads=[kod], writes=[ko])
            P.dma("sp", o_d[q0:q0 + 128, 768:1024], o[:, 512:768], reads=[kod], writes=[ko])


TWO_PI = 2.0 * math.pi


def sincos(P, k, ang, shape, out_s, out_c, T, key_in, key_out):
    r, ri, rf, m = T["r"], T["ri"], T["rf"], T["m"]
    sl = tuple(slice(0, n) for n in shape)
    for off, out in ((0.5, out_s), (0.75, out_c)):
        k.ts("dve", r[sl], ang, 1.0 / TWO_PI, ALU.mult, reads=[key_in], writes=["sc_t"], s2=off, op1=ALU.add)
        k.copy("dve", ri[sl], r[sl], reads=["sc_t"], writes=["sc_t"])
        k.copy("dve", rf[sl], ri[sl], reads=["sc_t"], writes=["sc_t"])
        k.tt("dve", r[sl], r[sl], rf[sl], ALU.subtract, reads=["sc_t"], writes=["sc_t"])
        k.ts("dve", m[sl], r[sl], 0.0, ALU.is_lt, reads=["sc_t"], writes=["sc_t"])
        k.tt("dve", r[sl], r[sl], m[sl], ALU.add, reads=["sc_t"], writes=["sc_t"])
        k.ts("dve", m[sl], r[sl], 1.0, ALU.is_ge, reads=["sc_t"], writes=["sc_t"])
        k.tt("dve", r[sl], r[sl], m[sl], ALU.subtract, reads=["sc_t"], writes=["sc_t"])
        k.ts("dve", r[sl], r[sl], TWO_PI, ALU.mult, reads=["sc_t"], writes=["sc_t"], s2=-math.pi, op1=ALU.add)
        k.act(out, r[sl], AF.Sin, reads=["sc_t"], writes=[key_out])


def mixa3_stage(P, k, C, x1_d, kx1, o_d, ko, win_d, CD, S5D, wglu_d):
    P.stage_begin()
    banks = C["banks"]
    wc2 = [0]

    def wb():
        i = 4 + wc2[0] % 2
        wc2[0] += 1
        return banks[i], ("PW", i - 4)
    wsu = P.sb([128, 8, 256], BF16)
    load_w(P, wsu[:], win_d[:, 1572:1828], "w_a3")
    wglu = P.sb([128, 2, 512], BF16)
    P.dma("pool", wglu[:], wglu_d.rearrange("(c p) f -> p c f", p=128), writes=["wglu"])

    def ld(name, shape):
        t = P.sb(shape, F32)
        P.dma("sp", t[:], S5D[name], writes=[name])
        return t
    c_are = ld("s5c_are", [128, 2, 64])
    c_aim = ld("s5c_aim", [128, 2, 64])
    c_bre = ld("s5c_bre", [128, 2, 64])
    c_bim = ld("s5c_bim", [128, 2, 64])
    c_ls = ld("s5c_ls", [128, 2])
    c_d = ld("s5c_d", [128, 2])
    s_are = ld("s5s_are", [128, 8])
    s_aim = ld("s5s_aim", [128, 8])
    s_ls = ld("s5s_ls", [128, 8])
    s_cre = ld("s5s_cre", [128, 8, 16])
    s_cim = ld("s5s_cim", [128, 8, 16])
    MSK = P.sb([128, 2], F32)
    P.dma("sp", MSK[:], CD["c_msk2"], writes=["MSK"])
    RM = P.sb([128, 4], F32)
    P.dma("sp", RM[:], CD["c_rm"], writes=["RM"])
    IOTA = P.sb([128, 256], F32)
    P.dma("sp", IOTA[:], CD["c_iota"], writes=["IOTA"])
    T = {"r": P.sb([128, 256], F32), "ri": P.sb([128, 256], I32), "rf": P.sb([128, 256], F32),
         "m": P.sb([128, 256], F32)}

    def f32(shape):
        return P.sb(shape, F32)
    dtc = f32([128, 2])
    k.act(dtc[:], c_ls[:], AF.Exp, reads=["s5c_ls"], writes=["dtc"])
    ar, ai, mag, sn, cs = f32([128, 128]), f32([128, 128]), f32([128, 128]), f32([128, 128]), f32([128, 128])
    are2 = c_are[:].rearrange("p a b -> p (a b)")
    aim2 = c_aim[:].rearrange("p a b -> p (a b)")
    bre2 = c_bre[:].rearrange("p a b -> p (a b)")
    bim2 = c_bim[:].rearrange("p a b -> p (a b)")
    for c2 in range(2):
        cs_ = slice(64 * c2, 64 * c2 + 64)
        k.ts("dve", ar[:, cs_], c_are[:, c2, :], dtc[:, c2:c2 + 1], ALU.mult, reads=["s5c_are", "dtc"], writes=["ar"])
        k.ts("dve", ai[:, cs_], c_aim[:, c2, :], dtc[:, c2:c2 + 1], ALU.mult, reads=["s5c_aim", "dtc"], writes=["ai"])
    k.act(mag[:], ar[:], AF.Exp, reads=["ar"], writes=["mag"])
    sincos(P, k, ai[:], [128, 128], sn[:], cs[:], T, "ai", "sncs")
    abr, abi, nr, den, t0, t1, cr, ci = [f32([128, 128]) for _ in range(8)]
    k.tt("dve", abr[:], mag[:], cs[:], ALU.mult, reads=["mag", "sncs"], writes=["abr"])
    k.tt("dve", abi[:], mag[:], sn[:], ALU.mult, reads=["mag", "sncs"], writes=["abi"])
    k.ts("dve", nr[:], abr[:], -1.0, ALU.add, reads=["abr"], writes=["nr"])
    k.tt("dve", den[:], are2, are2, ALU.mult, reads=["s5c_are"], writes=["den"])
    k.tt("dve", t0[:], aim2, aim2, ALU.mult, reads=["s5c_aim"], writes=["t0"])
    k.tt("dve", den[:], den[:], t0[:], ALU.add, reads=["den", "t0"], writes=["den"])
    P.op("dve", lambda e: e.reciprocal(out=den[:], in_=den[:]), reads=["den"], writes=["den"])
    k.tt("dve", t0[:], nr[:], are2, ALU.mult, reads=["nr", "s5c_are"], writes=["t0"])
    k.tt("dve", t1[:], abi[:], aim2, ALU.mult, reads=["abi", "s5c_aim"], writes=["t1"])
    k.tt("dve", t0[:], t0[:], t1[:], ALU.add, reads=["t0", "t1"], writes=["t0"])
    k.tt("dve", cr[:], t0[:], den[:], ALU.mult, reads=["t0", "den"], writes=["cr"])
    k.tt("dve", t0[:], abi[:], are2, ALU.mult, reads=["abi", "s5c_are"], writes=["t0"])
    k.tt("dve", t1[:], nr[:], aim2, ALU.mult, reads=["nr", "s5c_aim"], writes=["t1"])
    k.tt("dve", t0[:], t0[:], t1[:], ALU.subtract, reads=["t0", "t1"], writes=["t0"])
    k.tt("dve", ci[:], t0[:], den[:], ALU.mult, reads=["t0", "den"], writes=["ci"])
    Bre, Bim = f32([128, 128]), f32([128, 128])
    k.tt("dve", t0[:], cr[:], bre2, ALU.mult, reads=["cr", "s5c_bre"], writes=["t0"])
    k.tt("dve", t1[:], ci[:], bim2, ALU.mult, reads=["ci", "s5c_bim"], writes=["t1"])
    k.tt("dve", Bre[:], t0[:], t1[:], ALU.subtract, reads=["t0", "t1"], writes=["Bre"])
    k.tt("dve", t0[:], cr[:], bim2, ALU.mult, reads=["cr", "s5c_bim"], writes=["t0"])
    k.tt("dve", t1[:], ci[:], bre2, ALU.mult, reads=["ci", "s5c_bre"], writes=["t1"])
    k.tt("dve", Bim[:], t0[:], t1[:], ALU.add, reads=["t0", "t1"], writes=["Bim"])
    Bfull = f32([128, 2, 2, 128])
    for c2 in range(2):
        for ri_, src in ((0, Bre), (1, Bim)):
            for hf in range(2):
                k.ts("dve", Bfull[:, c2, ri_, 64 * hf:64 * hf + 64], src[:, 64 * c2:64 * c2 + 64], MSK[:, hf:hf + 1],
                     ALU.mult, reads=["Bre", "Bim", "MSK"], writes=["Bfull"])
    BW = P.sb([128, 8, 2, 128], BF16)
    for i in range(8):
        for ri_ in range(2):
            k.ts("dve", BW[:, i, ri_, :], Bfull[:, i // 4, ri_, :], RM[:, i % 4:i % 4 + 1], ALU.mult,
                 reads=["Bfull", "RM"], writes=["BW"])
    dts, mags, th = f32([128, 8]), f32([128, 8]), f32([128, 8])
    k.act(dts[:], s_ls[:], AF.Exp, reads=["s5s_ls"], writes=["dts"])
    k.tt("dve", mags[:], s_are[:], dts[:], ALU.mult, reads=["s5s_are", "dts"], writes=["mags"])
    k.act(mags[:], mags[:], AF.Exp, reads=["mags"], writes=["mags"])
    k.tt("dve", th[:], s_aim[:], dts[:], ALU.mult, reads=["s5s_aim", "dts"], writes=["th"])
    SINT = P.sb([128, 8, 256], F32)
    COST = P.sb([128, 8, 256], F32)
    angt = f32([128, 256])
    for i in range(8):
        k.ts("dve", angt[:], IOTA[:], th[:, i:i + 1], ALU.mult, reads=["IOTA", "th"], writes=["angt"])
        sincos(P, k, angt[:], [128, 256], SINT[:, i, :], COST[:, i, :], T, "angt", "tabs")
    ROTS, ROTC, a256 = f32([128, 8]), f32([128, 8]), f32([128, 8])
    k.ts("dve", a256[:], th[:], 256.0, ALU.mult, reads=["th"], writes=["a256"])
    sincos(P, k, a256[:], [128, 8], ROTS[:], ROTC[:], T, "a256", "rot")
    CW = P.sb([128, 8, 4, 128], BF16)
    k.memset("pool", CW[:].rearrange("p a b c -> p (a b c)"), 0.0, writes=["CW"])
    for i in range(8):
        r_ = i % 4
        for hf in range(2):
            ps_ = slice(64 * hf, 64 * hf + 64)
            cols = slice(32 * r_ + 16 * hf, 32 * r_ + 16 * hf + 16)
            k.copy("dve", CW[ps_, i, 0, cols], s_cre[ps_, i, :], reads=["s5s_cre", "CW"], writes=["CW"])
            k.ts("dve", CW[ps_, i, 1, cols], s_cre[ps_, i, :], -1.0, ALU.mult, reads=["s5s_cre", "CW"], writes=["CW"])
            k.ts("dve", CW[ps_, i, 2, cols], s_cim[ps_, i, :], -1.0, ALU.mult, reads=["s5s_cim", "CW"], writes=["CW"])
            k.ts("dve", CW[ps_, i, 3, cols], s_cim[ps_, i, :], -1.0, ALU.mult, reads=["s5s_cim", "CW"], writes=["CW"])
    qi_re, qi_im = f32([128, 8]), f32([128, 8])
    k.memset("dve", qi_re[:], 0.0, writes=["qi"])
    k.memset("dve", qi_im[:], 0.0, writes=["qi"])
    hd = f32([128, 4])
    xt = [P.sb([128, D], F32) for _ in range(2)]
    xT = P.sb([128, 8, 512], BF16)
    uT = P.sb([128, 2, 512], BF16)
    du = f32([128, 2, 512])
    tA, tB, mre, mim, qre, qim = [f32([128, 256]) for _ in range(6)]
    PV = [P.sb([128, 256], BF16) for _ in range(4)]
    yb, y2, sg = f32([128, 256]), f32([128, 256]), f32([128, 256])
    gT = P.sb([128, 2, 256], BF16)
    sgt = f32([128, 256])
    OC = [f32([128, 256]) for _ in range(2)]

    for G in range(SEQ // 512):
        c0 = G * 512
        for tt in range(4):
            b = k.rot("a3xt", 2)
            P.dma("sp", xt[b][:], x1_d[c0 + tt * 128:c0 + (tt + 1) * 128, :], reads=[kx1], writes=[("xt", b)])
            load_transposed(P, k, C, xt[b], ("xt", b), xT, "xT", tt * 128)
        for c2 in range(2):
            pb, kpb = wb()
            for kc in range(8):
                k.mm(pb[:], wsu[:, kc, c2 * 128:(c2 + 1) * 128], xT[:, kc, :], kc == 0, kc == 7,
                     reads=["xT", "w_a3"], writes=[kpb])
            k.copy("act", uT[:, c2, :], pb[:], reads=[kpb], writes=["uT"])
            k.ts("dve", du[:, c2, :], pb[:], c_d[:, c2:c2 + 1], ALU.mult, reads=[kpb, "s5c_d"], writes=["du"])
        for sb_ in range(2):
            cs_ = slice(sb_ * 256, (sb_ + 1) * 256)
            for i in range(8):
                c2 = i // 4
                bu, kbu = wb()
                for ri_ in range(2):
                    k.mm(bu[:, 256 * ri_:256 * ri_ + 256], BW[:, i, ri_, :], uT[:, c2, cs_], True, True,
                         reads=["BW", "uT"], writes=[kbu])
                Ct = COST[:, i, :]
                St = SINT[:, i, :]
                k.tt("dve", tA[:], Ct, bu[:, 0:256], ALU.mult, reads=["tabs", kbu], writes=["tA"])
                k.tt("dve", tB[:], St, bu[:, 256:512], ALU.mult, reads=["tabs", kbu], writes=["tB"])
                k.tt("pool", mre[:], tA[:], tB[:], ALU.add, reads=["tA", "tB"], writes=["mre"])
                k.tt("dve", tA[:], Ct, bu[:, 256:512], ALU.mult, reads=["tabs", kbu, "tA"], writes=["tA"])
                k.tt("dve", tB[:], St, bu[:, 0:256], ALU.mult, reads=["tabs", kbu, "tB"], writes=["tB"])
                k.tt("pool", mim[:], tA[:], tB[:], ALU.subtract, reads=["tA", "tB"], writes=["mim"])
                P.op("dve", (lambda i: lambda e: e.tensor_tensor_scan(
                    out=qre[:], data0=mags[:, i:i + 1].to_broadcast([128, 256]), data1=mre[:],
                    initial=qi_re[:, i:i + 1], op0=ALU.mult, op1=ALU.add))(i),
                    reads=["mags", "mre", "qi"], writes=["qre"])
                P.op("dve", (lambda i: lambda e: e.tensor_tensor_scan(
                    out=qim[:], data0=mags[:, i:i + 1].to_broadcast([128, 256]), data1=mim[:],
                    initial=qi_im[:, i:i + 1], op0=ALU.mult, op1=ALU.add))(i),
                    reads=["mags", "mim", "qi"], writes=["qim"])
                a_ = qre[:, 255:256]
                b_ = qim[:, 255:256]
                k.tt("pool", hd[:, 0:1], a_, ROTC[:, i:i + 1], ALU.mult, reads=["qre", "rot"], writes=["hd"])
                k.tt("pool", hd[:, 1:2], b_, ROTS[:, i:i + 1], ALU.mult, reads=["qim", "rot"], writes=["hd"])
                k.tt("pool", hd[:, 2:3], a_, ROTS[:, i:i + 1], ALU.mult, reads=["qre", "rot"], writes=["hd"])
                k.tt("pool", hd[:, 3:4], b_, ROTC[:, i:i + 1], ALU.mult, reads=["qim", "rot"], writes=["hd"])
                k.tt("pool", qi_re[:, i:i + 1], hd[:, 0:1], hd[:, 1:2], ALU.subtract, reads=["hd", "qi"], writes=["qi"])
                k.tt("pool", qi_im[:, i:i + 1], hd[:, 2:3], hd[:, 3:4], ALU.add, reads=["hd", "qi"], writes=["qi"])
                k.tt("pool", PV[0][:], qre[:], Ct, ALU.mult, reads=["qre", "tabs"], writes=[("PV", 0)])
                k.tt("pool", PV[1][:], qim[:], St, ALU.mult, reads=["qim", "tabs"], writes=[("PV", 1)])
                k.tt("dve", PV[2][:], qre[:], St, ALU.mult, reads=["qre", "tabs"], writes=[("PV", 2)])
                k.tt("dve", PV[3][:], qim[:], Ct, ALU.mult, reads=["qim", "tabs"], writes=[("PV", 3)])
                for v in range(4):
                    k.mm(banks[c2][:, 0:256], CW[:, i, v, :], PV[v][:], i % 4 == 0 and v == 0, i % 4 == 3 and v == 3,
                         reads=["CW", ("PV", v)], writes=[("ACC", c2)])
            for c2 in range(2):
                k.tt("dve", yb[:], du[:, c2, cs_], banks[c2][:, 0:256], ALU.add, reads=["du", ("ACC", c2)], writes=["yb"])
                k.act(y2[:], yb[:], AF.Square, reads=["yb"], writes=["y2"])
                k.ts("pool", y2[:], y2[:], 0.0713548162726, ALU.mult, reads=["y2"], writes=["y2"], s2=1.59576912161,
                     op1=ALU.add)
                k.tt("pool", y2[:], y2[:], yb[:], ALU.mult, reads=["y2", "yb"], writes=["y2"])
                k.act(sg[:], y2[:], AF.Sigmoid, reads=["y2"], writes=["sg"])
                k.tt("pool", gT[:, c2, :], yb[:], sg[:], ALU.mult, reads=["yb", "sg"], writes=["gT"])
            for t2 in range(2):
                T_ = 4 * G + 2 * sb_ + t2
                gl, kgl = wb()
                for c2 in range(2):
                    k.mm(gl[:], gT[:, c2, t2 * 128:(t2 + 1) * 128], wglu[:, c2, :], c2 == 0, c2 == 1,
                         reads=["gT", "wglu"], writes=[kgl])
                k.act(sgt[:], gl[:, 256:512], AF.Sigmoid, reads=[kgl], writes=["sgt"])
                oc = OC[T_ % 2]
                k.tt("dve", oc[:], sgt[:], gl[:, 0:256], ALU.mult, reads=["sgt", kgl], writes=[("OC", T_ % 2)])
                P.dma("sp", o_d[T_ * 128:(T_ + 1) * 128, 512:768], oc[:], reads=[("OC", T_ % 2)], writes=[ko])


WNAMES = ("ln_g", "ln_b", "ffn_wi", "ffn_wo", "w_in", "w_branch", "w_out", "ret_gn", "mla_q_norm", "mla_kv_norm",
          "mla_w_uq", "mla_w_ukv")


def build_program(plan, ext=()):
    nc = bass.Bass("TRN2", target_bir_lowering=False)

    def din(name, shape, dtype=F32):
        return nc.dram_tensor(name, list(shape), dtype, kind="ExternalInput").ap()

    x = din("x", [SEQ, D])
    ln_g = din("ln_g", [DEPTH, 3, D])
    ln_b = din("ln_b", [DEPTH, 3, D])
    ffn_wi = din("ffn_wi", [DEPTH, 2, D, 2 * DFF])
    ffn_wo = din("ffn_wo", [DEPTH, 2, DFF, D])
    w_in = din("w_in", [DEPTH, D, NIN])
    w_branch = din("w_branch", [DEPTH, 4, 256, D])
    w_out = din("w_out", [DEPTH, D, D])
    ret_gn = din("ret_gn", [DEPTH, 256])
    mla_q_norm = din("mla_q_norm", [DEPTH, 256])
    mla_kv_norm = din("mla_kv_norm", [DEPTH, 128])
    mla_w_uq = din("mla_w_uq", [DEPTH, 256, 384])
    mla_w_ukv = din("mla_w_ukv", [DEPTH, 128, 512])
    s5_w_glu = din("s5_w_glu", [DEPTH, 256, 512])
    S5IN = {n: din(n, [DEPTH] + shp) for n, shp in S5_SHAPES.items()}
    CD = {}
    for nm, shp in CONST_SHAPES.items():
        CD[nm] = din(nm, shp)
    c_ident = CD["c_ident"]
    out = nc.dram_tensor("out", [SEQ, D], F32, kind="ExternalOutput").ap()
    S = {}
    for nm in ("s_x1", "s_o", "s_x2", "s_x3"):
        S[nm] = nc.dram_tensor(nm, [SEQ, D], F32, kind="ExternalInput" if nm in ext else "Internal").ap()

    P = Prog(nc)
    k = K(P)
    C = {}
    C["ident"] = P.sb([128, 128], F32)
    P.dma("sp", C["ident"][:], c_ident, writes=["ident"])
    P.persist()
    banks = [P.ps([128, 512], F32) for _ in range(6)]
    C["ACC"] = banks[0:4]
    C["PW"] = banks[4:6]
    C["banks"] = banks
    bbanks = [P.ps([128, 1024], BF16) for _ in range(2)]
    wc = [0]

    def wbank():
        i = 2 + wc[0] % 4
        wc[0] += 1
        return banks[i], ("ACC", i) if i < 4 else ("PW", i - 4)
    C["wbank"] = wbank
    bbc = [0]

    def bbank():
        i = bbc[0] % 2
        bbc[0] += 1
        return bbanks[i], ("BB", i)
    C["bbank"] = bbank
    bc = [0]

    def bank():
        i = bc[0] % 6
        bc[0] += 1
        return banks[i], ("ACC", i) if i < 4 else ("PW", i - 4)
    C["bank"] = bank

    nplan = len(plan)
    for pi, (st, l) in enumerate(plan):
        last = pi == nplan - 1
        if st == "ffn1":
            src, ks = (x, "x") if l == 0 else (S["s_x3"], "s_x3")
            dst, kd = (out, "out") if last else (S["s_x1"], "s_x1")
            ffn_stage(P, k, C, src, ks, dst, kd, ffn_wi[l, 0], ffn_wo[l, 0], ln_g[l, 0], ln_b[l, 0])
        elif st == "mixb":
            dst, kd = (out, "out") if last else (S["s_x2"], "s_x2")
            mixb_stage(P, k, C, S["s_x1"], "s_x1", S["s_o"], "s_o", dst, kd, w_in[l], w_branch[l], w_out[l],
                       ln_g[l, 1], ln_b[l, 1])
        elif st == "mixa1":
            od, kod = (out, "out") if last else (S["s_o"], "s_o")
            mixa1_stage(P, k, C, S["s_x1"], "s_x1", od, kod, w_in[l], CD)
        elif st in ("mixa2", "mixa2r", "mixa2m", "mixa2n"):
            od, kod = (out, "out") if last else (S["s_o"], "s_o")
            mixa2_stage(P, k, C, S["s_x1"], "s_x1", od, kod, w_in[l], CD, ret_gn[l], mla_q_norm[l], mla_kv_norm[l],
                        mla_w_uq[l], mla_w_ukv[l], do_ret=st in ("mixa2", "mixa2r"), do_mla=st in ("mixa2", "mixa2m"))
        elif st == "mixa3":
            od, kod = (out, "out") if last else (S["s_o"], "s_o")
            mixa3_stage(P, k, C, S["s_x1"], "s_x1", od, kod, w_in[l], CD, {n: a[l] for n, a in S5IN.items()}, s5_w_glu[l])
        elif st == "ffn2":
            dst, kd = (out, "out") if last else (S["s_x3"], "s_x3")
            ffn_stage(P, k, C, S["s_x2"], "s_x2", dst, kd, ffn_wi[l, 1], ffn_wo[l, 1], ln_g[l, 2], ln_b[l, 2])
        else:
            raise ValueError(st)
    P.finish()
    P.build()
    print("ops", P.n_ops, "sbuf max", P.sb_max, flush=True)
    return nc


S5_SHAPES = {
    "s5c_are": [128, 2, 64], "s5c_aim": [128, 2, 64], "s5c_bre": [128, 2, 64], "s5c_bim": [128, 2, 64],
    "s5c_ls": [128, 2], "s5c_d": [128, 2],
    "s5s_are": [128, 8], "s5s_aim": [128, 8], "s5s_ls": [128, 8],
    "s5s_cre": [128, 8, 16], "s5s_cim": [128, 8, 16],
}


def s5_layouts(inputs):
    f = lambda n: np.asarray(inputs[n], dtype=np.float32)
    a_re, a_im, b_re, b_im = f("s5_a_re"), f("s5_a_im"), f("s5_b_re"), f("s5_b_im")
    c_re, c_im, d_, ls = f("s5_c_re"), f("s5_c_im"), f("s5_d"), f("s5_log_step")
    L = a_re.shape[0]
    p = np.arange(128)
    out = {}
    gl, c = p // 16, p % 16
    g_c = (np.arange(2)[None, :] * 8 + gl[:, None])
    out["s5c_are"] = a_re[:, g_c, :]
    out["s5c_aim"] = a_im[:, g_c, :]
    out["s5c_bre"] = np.stack([b_re[:, g_c[:, j], :, c] for j in range(2)], axis=0).transpose(2, 1, 0, 3) \
        if False else np.stack([np.stack([b_re[l][g_c[:, j], :, c] for j in range(2)], axis=1) for l in range(L)], 0)
    out["s5c_bim"] = np.stack([np.stack([b_im[l][g_c[:, j], :, c] for j in range(2)], axis=1) for l in range(L)], 0)
    out["s5c_ls"] = ls[:, g_c]
    out["s5c_d"] = np.stack([np.stack([d_[l][g_c[:, j], c] for j in range(2)], axis=1) for l in range(L)], 0)
    gl2, s_ = p // 64, p % 64
    g_s = 2 * np.arange(8)[None, :] + gl2[:, None]
    out["s5s_are"] = np.stack([a_re[l][g_s, s_[:, None]] for l in range(L)], 0)
    out["s5s_aim"] = np.stack([a_im[l][g_s, s_[:, None]] for l in range(L)], 0)
    out["s5s_ls"] = ls[:, g_s]
    out["s5s_cre"] = np.stack([c_re[l][g_s, :, s_[:, None]] for l in range(L)], 0)
    out["s5s_cim"] = np.stack([c_im[l][g_s, :, s_[:, None]] for l in range(L)], 0)
    return {k_: np.ascontiguousarray(v.astype(np.float32)) for k_, v in out.items()}


CONST_SHAPES = {
    "c_ident": [128, 128],
    "c_kpos": [2, SEQ],
    "c_qaug": [4, 2, 512],
    "c_bq": [128, 32, 4],
    "c_corr": [128, 4, 128],
    "c_pw2": [128, 2, NIT],
    "c_negidx": [128, SEQ],
    "c_qpos": [128, 32],
    "c_nsl": [128, 4],
    "c_xi": [128, 2, 512],
    "c_zeta": [128, 256],
    "c_dect": [128, 512],
    "c_gch": [128, 128],
    "c_cmt": [128, 512],
    "c_cos": [128, 32, 16],
    "c_sin": [128, 32, 16],
    "c_msk2": [128, 2],
    "c_rm": [128, 4],
    "c_iota": [128, 256],
}


def make_consts():
    c = {}
    c["c_ident"] = np.eye(128, dtype=np.float32)
    pos = np.arange(SEQ)
    c["c_kpos"] = np.stack([pos // 64, pos % 64]).astype(np.float32)
    qa = np.zeros((4, 2, 512), np.float32)
    for h in range(4):
        qa[h, 0, :] = 512.0 * SLOPES[h]
        qa[h, 1, :] = 8.0 * SLOPES[h]
    c["c_qaug"] = qa
    p = np.arange(128)[:, None, None]
    T = np.arange(32)[None, :, None]
    sl = np.array(SLOPES, np.float64)[None, None, :]
    c["c_bq"] = (-(sl) * (128 * T + p)).astype(np.float32)
    qq = np.arange(128)[:, None]
    kk = np.arange(128)[None, :]
    corr = np.ones((128, 4, 128), np.float64)
    for h in range(4):
        corr[:, h, :] = np.where(kk > qq, -2.0 * SLOPES[h] * (kk - qq), 0.0)
    c["c_corr"] = corr.astype(np.float32)
    pw = np.zeros((128, 2, NIT), np.float32)
    for it in range(NIT):
        pw[:, 0, it] = 2.0 ** (-(it + 1))
        pw[:, 1, it] = -(2.0 ** (-(it + 2)))
    c["c_pw2"] = pw
    lg = np.log1p(-np.exp2(-5.0 - np.arange(4, dtype=np.float64)))
    rows = np.arange(128)
    xi = np.zeros((128, 2, 512))
    for t in range(2):
        hh = 2 * t + rows // 64
        n = np.arange(512) % 128
        xi[:, t, :] = np.exp(lg[hh][:, None] * (n[None, :] + 1.0)) / 8.0
    c["c_xi"] = xi.astype(np.float32)
    m = rows
    zeta = np.zeros((128, 256))
    dect = np.zeros((128, 512))
    nn = np.arange(128)
    same = (m[:, None] // 64) == (nn[None, :] // 64)
    cross = (m[:, None] < 64) & (nn[None, :] >= 64)
    for h in range(4):
        zeta[:, 64 * h:64 * h + 64] = np.exp(lg[h] * (127.0 - m))[:, None]
        dd = np.where(same, np.exp(lg[h] * np.abs(nn[None, :] - m[:, None])), 0.0)
        dd = np.where(cross, np.exp(lg[h] * (nn[None, :] - m[:, None])), dd)
        dect[:, 128 * h:128 * h + 128] = dd / 8.0
    c["c_zeta"] = zeta.astype(np.float32)
    c["c_dect"] = dect.astype(np.float32)
    gch = np.zeros((128, 128))
    for t in range(2):
        hh = 2 * t + rows // 64
        gch[:, 64 * t:64 * t + 64] = np.exp(lg[hh] * 128.0)[:, None]
    c["c_gch"] = gch.astype(np.float32)
    kq = np.arange(128)
    cm = np.ones((128, 128), np.float32)
    cm[64:, :64] = 0.0
    c["c_cmt"] = np.tile(cm, (1, 4))
    pos = (128.0 * np.arange(32)[None, :] + np.arange(128)[:, None])
    fr = 10000.0 ** (-np.arange(16, dtype=np.float64) / 16.0)
    ang = (pos[:, :, None].astype(np.float32) * fr[None, None, :].astype(np.float32)).astype(np.float32)
    c["c_cos"] = np.cos(ang).astype(np.float32)
    c["c_sin"] = np.sin(ang).astype(np.float32)
    pp = np.arange(128)
    c["c_msk2"] = np.stack([((pp // 16) % 2 == 0), ((pp // 16) % 2 == 1)], axis=1).astype(np.float32)
    c["c_rm"] = (pp[:, None] // 32 == np.arange(4)[None, :]).astype(np.float32)
    c["c_iota"] = np.broadcast_to(np.arange(256, dtype=np.float32)[None, :], (128, 256)).copy()
    c["c_qpos"] = (128.0 * np.arange(32)[None, :] + np.arange(128)[:, None]).astype(np.float32)
    c["c_nsl"] = np.broadcast_to(-np.array(SLOPES, np.float32)[None, :], (128, 4)).copy()
    c["c_negidx"] = np.broadcast_to(-(np.arange(SEQ, dtype=np.float32) * np.float32(2.0 ** -12))[None, :], (128, SEQ)).copy()
    return c


def make_shared(inputs):
    shared = {}
    for name in WNAMES:
        shared[name] = np.ascontiguousarray(np.asarray(inputs[name], dtype=np.float32))
    shared["s5_w_glu"] = np.ascontiguousarray(np.asarray(inputs["s5_w_glu"], dtype=np.float32))
    shared.update(s5_layouts(inputs))
    shared.update(make_consts())
    return shared


FULL_PLAN = [(st, l) for l in range(DEPTH) for st in ("ffn1", "mixa1", "mixa2", "mixa3", "mixb", "ffn2")]


def kernel(**inputs):
    ncores = 8
    nc = build_program(FULL_PLAN)
    xs = np.ascontiguousarray(np.asarray(inputs["x"], dtype=np.float32))
    shared = make_shared(inputs)
    in_maps = []
    for c in range(ncores):
        m = dict(shared)
        m["x"] = xs[c % 4]
        in_maps.append(m)
    res = run_bass_kernel_spmd(nc, in_maps, core_ids=list(range(ncores)))
    outs = [res.results[c]["out"] for c in range(4)]
    return np.stack(outs, axis=0).astype(np.float32)
```

```python
from contextlib import ExitStack
import math
import numpy as np
import concourse.bass as bass
import concourse.mybir as mybir
from concourse.bass_utils import run_bass_kernel_spmd

F32 = mybir.dt.float32
BF16 = mybir.dt.bfloat16
I32 = mybir.dt.int32
U32 = mybir.dt.uint32
AF = mybir.ActivationFunctionType
ALU = mybir.AluOpType
AX = mybir.AxisListType

NDMA = 24
SEQ = 4096
D = 1024
DFF = 2816
DEPTH = 2
ALPHA = (2 * DEPTH) ** 0.25
LN_EPS = 1e-5
RMS_EPS = 1e-6
NIN = 6340
NSM = 2244
BIG = 1.0e30


class Prog:
    ENG = ("pe", "act", "dve", "pool", "sp")

    def __init__(self, nc):
        self.nc = nc
        self.es = ExitStack()
        self.q = {e: [] for e in self.ENG}
        self.cnt = {e: 0 for e in self.ENG}
        self.sems = {}
        for e in self.ENG:
            self.sems[e] = self.es.enter_context(nc.semaphore("s_" + e))
        for i in range(NDMA):
            self.sems[("dma", i)] = self.es.enter_context(nc.semaphore("s_dma%d" % i))
        self.dma_cnt = [0] * NDMA
        self.dma_rr = 0
        self.known = {e: {} for e in self.ENG}
        self.last_w = {}
        self.readers = {}
        self.ntile = 0
        self.n_ops = 0
        self.sb_off = 16384 + 64
        self.sb_base = 16384 + 64
        self.sb_max = 0

    def sb(self, shape, dtype, name=None):
        self.ntile += 1
        name = name or ("t%d" % self.ntile)
        esz = 4 if dtype in (F32, I32, U32) else 2
        n = 1
        for s in shape[1:]:
            n *= s
        nbytes = (n * esz + 63) // 64 * 64
        off = self.sb_off
        self.sb_off += nbytes
        self.sb_max = max(self.sb_max, self.sb_off)
        assert self.sb_off <= 229376 - 512, ("SBUF overflow", self.sb_off)
        return self.nc.alloc_sbuf_tensor_at(name, list(shape), dtype, offset=off)

    def ps(self, shape, dtype, name=None):
        self.ntile += 1
        name = name or ("p%d" % self.ntile)
        return self.es.enter_context(self.nc.psum_tensor(name, list(shape), dtype))

    def persist(self):
        self.sb_base = self.sb_off

    def stage_begin(self):
        self.barrier()
        self.sb_off = self.sb_base

    def _deps(self, eng, reads, writes):
        w = {}

        def add(ev):
            if ev is None:
                return
            k, v = ev
            if k == "pe" and eng == "pe":
                return
            if self.known[eng].get(k, 0) >= v:
                return
            if w.get(k, 0) < v:
                w[k] = v

        for k in reads:
            add(self.last_w.get(k))
            if isinstance(k, tuple) and k[0] in ("ACC", "PW", "BB"):
                for ev in self.readers.get(k, {}).items():
                    if ev[0] != eng:
                        add(ev)
        for k in writes:
            add(self.last_w.get(k))
            for ev in self.readers.get(k, {}).items():
                add(ev)
        for k, v in w.items():
            self.known[eng][k] = v
        return list(w.items())

    def _commit(self, ev, reads, writes):
        for k in writes:
            self.last_w[k] = ev
            self.readers[k] = {}
        for k in reads:
            r = self.readers.setdefault(k, {})
            if r.get(ev[0], 0) < ev[1]:
                r[ev[0]] = ev[1]

    def op(self, eng, emit, reads=(), writes=()):
        waits = self._deps(eng, reads, writes)
        self.cnt[eng] += 1
        idx = self.cnt[eng]
        self.q[eng].append((waits, emit, True))
        self._commit((eng, idx), reads, writes)
        self.n_ops += 1

    def dma(self, qeng, out, in_, reads=(), writes=(), **kw):
        s = self.dma_rr
        self.dma_rr = (s + 1) % NDMA
        waits = self._deps(qeng, reads, writes)
        prev = self.dma_cnt[s] * 16
        key = ("dma", s)
        if prev > 0 and self.known[qeng].get(key, 0) < prev:
            waits.append((key, prev))
            self.known[qeng][key] = prev
        self.dma_cnt[s] += 1
        tgt = self.dma_cnt[s] * 16
        sem = self.sems[key]

        def emit(e, out=out, in_=in_, sem=sem, kw=kw):
            e.dma_start(out=out, in_=in_, **kw).then_inc(sem, 16)
            return None

        self.q[qeng].append((waits, emit, False))
        self._commit((key, tgt), reads, writes)
        self.n_ops += 1

    def barrier(self):
        evs = []
        for s in range(NDMA):
            if self.dma_cnt[s] > 0:
                evs.append((("dma", s), self.dma_cnt[s] * 16))
        for e in ("pe", "act", "dve", "pool"):
            if self.cnt[e] > 0:
                evs.append((e, self.cnt[e]))
        for e in self.ENG:
            waits = []
            for k, v in evs:
                if k == e and e == "pe":
                    continue
                if self.known[e].get(k, 0) < v:
                    waits.append((k, v))
                    self.known[e][k] = v
            if waits:
                self.q[e].append((waits, None, False))

    def finish(self):
        self.barrier()

    def build(self):
        nc = self.nc
        sems = self.sems
        q = self.q

        def replay(name, eng):
            mysem = sems[name]
            for waits, emit, inc in q[name]:
                for k, v in waits:
                    eng.wait_ge(sems[k], v)
                if emit is None:
                    continue
                ins = emit(eng)
                if inc:
                    ins.then_inc(mysem, 1)

        with nc.Block() as block:
            @block.tensor
            def _(e):
                replay("pe", e)

            @block.scalar
            def _(e):
                replay("act", e)

            @block.vector
            def _(e):
                replay("dve", e)

            @block.gpsimd
            def _(e):
                replay("pool", e)

            @block.sync
            def _(e):
                replay("sp", e)
        self.es.close()


class K:
    def __init__(self, P):
        self.P = P
        self.rr = {}

    def mm(self, out, lhsT, rhs, start, stop, reads, writes):
        self.P.op("pe", lambda e: e.matmul(out, lhsT=lhsT, rhs=rhs, start=start, stop=stop,
                                           skip_group_check=True), reads=reads, writes=writes)

    def tr(self, out, in_, ident, reads, writes):
        self.P.op("pe", lambda e: e.transpose(out, in_, ident), reads=reads, writes=writes)

    def act(self, out, in_, func, reads, writes, scale=1.0, bias=0.0, accum_out=None, eng="act"):
        if accum_out is None:
            self.P.op(eng, lambda e: e.activation(out=out, in_=in_, func=func, scale=scale, bias=bias),
                      reads=reads, writes=writes)
        else:
            self.P.op(eng, lambda e: e.activation(out=out, in_=in_, func=func, scale=scale, bias=bias,
                                                  accum_out=accum_out), reads=reads, writes=writes)

    def tt(self, eng, out, in0, in1, op, reads, writes):
        self.P.op(eng, lambda e: e.tensor_tensor(out=out, in0=in0, in1=in1, op=op), reads=reads, writes=writes)

    def ts(self, eng, out, in0, s1, op0, reads, writes, s2=None, op1=None, accum_out=None):
        if op1 is None:
            self.P.op(eng, lambda e: e.tensor_scalar(out=out, in0=in0, scalar1=s1, scalar2=None, op0=op0),
                      reads=reads, writes=writes)
        elif accum_out is None:
            self.P.op(eng, lambda e: e.tensor_scalar(out=out, in0=in0, scalar1=s1, scalar2=s2, op0=op0, op1=op1),
                      reads=reads, writes=writes)
        else:
            self.P.op(eng, lambda e: e.tensor_scalar(out=out, in0=in0, scalar1=s1, scalar2=s2, op0=op0, op1=op1,
                                                     accum_out=accum_out), reads=reads, writes=writes)

    def stt(self, out, in0, scalar, in1, op0, op1, reads, writes, accum_out=None):
        if accum_out is None:
            self.P.op("dve", lambda e: e.scalar_tensor_tensor(out=out, in0=in0, scalar=scalar, in1=in1,
                                                              op0=op0, op1=op1), reads=reads, writes=writes)
        else:
            self.P.op("dve", lambda e: e.scalar_tensor_tensor(out=out, in0=in0, scalar=scalar, in1=in1,
                                                              op0=op0, op1=op1, accum_out=accum_out),
                      reads=reads, writes=writes)

    def copy(self, eng, out, in_, reads, writes):
        if eng == "act":
            self.P.op("act", lambda e: e.activation(out=out, in_=in_, func=AF.Copy), reads=reads, writes=writes)
        else:
            self.P.op(eng, lambda e: e.tensor_copy(out=out, in_=in_), reads=reads, writes=writes)

    def memset(self, eng, ap, val, writes):
        self.P.op(eng, lambda e: e.memset(ap, val), writes=writes)

    def rot(self, name, n):
        i = self.rr.get(name, 0)
        self.rr[name] = (i + 1) % n
        return i


def bcast_rows(ap1d, n):
    return ap1d.unsqueeze(0).to_broadcast([128, n])


def layer_norm_tile(P, k, z, kz, gam, bet, eps, tmp):
    st, mv, rs = tmp["st"], tmp["mv"], tmp["rs"]
    kt = tmp["key"]
    for dh in range(2):
        P.op("dve", (lambda dh: lambda e: e.bn_stats(out=st[:, dh, :], in_=z[:, dh * 512:(dh + 1) * 512]))(dh),
             reads=[kz], writes=[kt])
    P.op("dve", lambda e: e.bn_aggr(out=mv[:], in_=st[:].rearrange("p a b -> p (a b)")), reads=[kt], writes=[kt])
    k.ts("dve", rs[:], mv[:, 1:2], eps, ALU.add, reads=[kt], writes=[kt])
    k.act(rs[:], rs[:], AF.Sqrt, reads=[kt], writes=[kt])
    P.op("dve", lambda e: e.reciprocal(out=rs[:], in_=rs[:]), reads=[kt], writes=[kt])
    k.ts("dve", z[:], z[:], mv[:, 0:1], ALU.subtract, reads=[kz, kt], writes=[kz], s2=rs[:, 0:1], op1=ALU.mult)
    k.tt("pool", z[:], z[:], gam[:], ALU.mult, reads=[kz, "lnp"], writes=[kz])
    k.tt("pool", z[:], z[:], bet[:], ALU.add, reads=[kz, "lnp"], writes=[kz])


def ffn_stage(P, k, C, src, ksrc, dst, kdst, wi_d, wo_d, g_d, b_d):
    P.stage_begin()
    ident = C["ident"]
    PW = C["PW"]
    ACC = C["ACC"]
    wi = P.sb([128, 8, 2 * DFF], BF16)
    wo = P.sb([128, 22, D], BF16)
    gam = P.sb([128, D], F32)
    bet = P.sb([128, D], F32)
    xt = [P.sb([128, D], F32) for _ in range(4)]
    xT = [P.sb([128, 8, 256], BF16) for _ in range(2)]
    sa = [P.sb([128, 256], F32) for _ in range(2)]
    hT = [P.sb([128, 256], BF16) for _ in range(3)]
    zt = [P.sb([128, D], F32) for _ in range(2)]
    st = P.sb([128, 2, 6], F32)
    mv = P.sb([128, 2], F32)
    rs = P.sb([128, 1], F32)
    tmp = {"st": st, "mv": mv, "rs": rs, "key": "lntmp"}

    P.dma("sp", gam[:], bcast_rows(g_d, D), writes=["lnp"])
    P.dma("sp", bet[:], bcast_rows(b_d, D), writes=["lnp"])
    wi_v = wi_d.rearrange("(kc p) f -> p kc f", p=128)
    for fb in range(6):
        c_lo = fb * 512
        c_hi = min(DFF, c_lo + 512)
        for hf in range(2):
            P.dma("pool", wi[:, :, hf * DFF + c_lo:hf * DFF + c_hi], wi_v[:, :, hf * DFF + c_lo:hf * DFF + c_hi],
                  writes=[("wi", fb, hf)])
        for fc in range(fb * 4, min(22, fb * 4 + 4)):
            P.dma("pool", wo[:, fc, :], wo_d[fc * 128:(fc + 1) * 128, :], writes=[("wo", fc)])

    NG = SEQ // 256
    pwc = [0]

    def next_pw():
        i = pwc[0] % 2
        pwc[0] += 1
        return PW[i], ("PW", i)

    for g in range(NG):
        r0 = g * 256
        xs = [xt[(2 * g + t) % 4] for t in range(2)]
        kxs = [("xt", (2 * g + t) % 4) for t in range(2)]
        for t in range(2):
            P.dma("sp", xs[t][:], src[r0 + t * 128:r0 + (t + 1) * 128, :], reads=[ksrc], writes=[kxs[t]])
        xTg = xT[g % 2]
        kxT = ("xT", g % 2)
        for t in range(2):
            for hf in range(2):
                pw, kpw = next_pw()
                for j in range(4):
                    kc = hf * 4 + j
                    k.tr(pw[:, j * 128:(j + 1) * 128], xs[t][:, kc * 128:(kc + 1) * 128], ident[:],
                         reads=[kxs[t], "ident"], writes=[kpw])
                k.copy("act", xTg[:, hf * 4:(hf + 1) * 4, t * 128:(t + 1) * 128],
                       pw[:].rearrange("p (a b) -> p a b", a=4), reads=[kpw], writes=[kxT])

        def h_mm(f):
            pw, kpw = next_pw()
            for hf in range(2):
                for kc in range(8):
                    k.mm(pw[:, hf * 256:(hf + 1) * 256], wi[:, kc, hf * DFF + f * 128: hf * DFF + (f + 1) * 128],
                         xTg[:, kc, :], kc == 0, kc == 7, reads=[kxT, ("wi", f // 4, hf)], writes=[kpw])
            s = sa[f % 2]
            h = hT[f % 3]
            k.act(s[:], pw[:, 0:256], AF.Silu, reads=[kpw], writes=[("sa", f % 2)])
            k.tt("dve", h[:], s[:], pw[:, 256:512], ALU.mult, reads=[("sa", f % 2), kpw], writes=[("hT", f % 3)])

        def o_mm(f):
            h = hT[f % 3]
            for t in range(2):
                for dh in range(2):
                    k.mm(ACC[t * 2 + dh][:], h[:, t * 128:(t + 1) * 128], wo[:, f, dh * 512:(dh + 1) * 512],
                         f == 0, f == 21, reads=[("hT", f % 3), ("wo", f)], writes=[("ACC", t * 2 + dh)])

        h_mm(0)
        for f in range(1, 22):
            h_mm(f)
            o_mm(f - 1)
        o_mm(21)

        for t in range(2):
            z = zt[t]
            kz = ("zt", t)
            for dh in range(2):
                k.stt(z[:, dh * 512:(dh + 1) * 512], xs[t][:, dh * 512:(dh + 1) * 512], 2.0 * ALPHA,
                      ACC[t * 2 + dh][:], ALU.mult, ALU.add, reads=[kxs[t], ("ACC", t * 2 + dh)], writes=[kz])
            layer_norm_tile(P, k, z, kz, gam, bet, 4.0 * LN_EPS, tmp)
            P.dma("sp", dst[r0 + t * 128:r0 + (t + 1) * 128, :], z[:], reads=[kz], writes=[kdst])


def load_transposed(P, k, C, xs, kxs, xTg, kxT, col0, ncols=128):
    ident = C["ident"]
    for hf in range(2):
        pw, kpw = C["bank"]()
        for j in range(4):
            kc = hf * 4 + j
            k.tr(pw[:, j * 128:(j + 1) * 128], xs[:, kc * 128:(kc + 1) * 128], ident[:],
                 reads=[kxs, "ident"], writes=[kpw])
        k.copy("act", xTg[:, hf * 4:(hf + 1) * 4, col0:col0 + 128],
               pw[:].rearrange("p (a b) -> p a b", a=4), reads=[kpw], writes=[kxT])


def mixb_stage(P, k, C, x1_d, kx1, o_d, ko, dst, kdst, win_d, wbr_d, wout_d, g_d, b_d):
    P.stage_begin()
    wg = P.sb([128, 8, 4096], BF16)
    wbr = P.sb([128, 8, D], BF16)
    wout = P.sb([128, 8, D], BF16)
    gam = P.sb([128, D], F32)
    bet = P.sb([128, D], F32)
    xt = [P.sb([128, D], F32) for _ in range(2)]
    ot = [P.sb([128, D], F32) for _ in range(2)]
    xT = [P.sb([128, 8, 128], BF16) for _ in range(2)]
    oT = [P.sb([128, 8, 128], BF16) for _ in range(2)]
    mT = [P.sb([128, 8, 128], BF16) for _ in range(2)]
    sg = [P.sb([128, 512], F32) for _ in range(2)]
    pr = [P.sb([128, 512], F32) for _ in range(2)]
    mg = [P.sb([128, D], F32) for _ in range(2)]
    zt = [P.sb([128, D], F32) for _ in range(2)]
    tmp = {"st": P.sb([128, 2, 6], F32), "mv": P.sb([128, 2], F32), "rs": P.sb([128, 1], F32), "key": "lntmp"}
    P.dma("sp", gam[:], bcast_rows(g_d, D), writes=["lnp"])
    P.dma("sp", bet[:], bcast_rows(b_d, D), writes=["lnp"])
    wg_v = win_d.rearrange("(kc p) f -> p kc f", p=128)
    for q4 in range(4):
        for hh in range(2):
            lo = q4 * 1024 + hh * 512
            P.dma("pool", wg[:, :, lo:lo + 512], wg_v[:, :, NSM + lo:NSM + lo + 512], writes=[("wg", q4, hh)])
    for n in range(4):
        for c2 in range(2):
            P.dma("pool", wbr[:, n * 2 + c2, :], wbr_d[n, c2 * 128:(c2 + 1) * 128, :], writes=["wbr"])
    for kc in range(8):
        P.dma("pool", wout[:, kc, :], wout_d[kc * 128:(kc + 1) * 128, :], writes=["wout"])
    for t in range(SEQ // 128):
        r0 = t * 128
        b = t % 2
        P.dma("sp", xt[b][:], x1_d[r0:r0 + 128, :], reads=[kx1], writes=[("xt", b)])
        P.dma("sp", ot[b][:], o_d[r0:r0 + 128, :], reads=[ko], writes=[("ot", b)])
        load_transposed(P, k, C, xt[b], ("xt", b), xT[b], ("xT", b), 0)
        load_transposed(P, k, C, ot[b], ("ot", b), oT[b], ("oT", b), 0)
        m = mg[b]
        km = ("mg", b)
        for dh in range(2):
            for n in range(4):
                gp, kgp = C["bank"]()
                for kc in range(8):
                    k.mm(gp[:], xT[b][:, kc, :], wg[:, kc, n * 1024 + dh * 512: n * 1024 + (dh + 1) * 512],
                         kc == 0, kc == 7, reads=[("xT", b), ("wg", n, dh)], writes=[kgp])
                up, kup = C["bank"]()
                for c2 in range(2):
                    k.mm(up[:], oT[b][:, n * 2 + c2, :], wbr[:, n * 2 + c2, dh * 512:(dh + 1) * 512],
                         c2 == 0, c2 == 1, reads=[("oT", b), "wbr"], writes=[kup])
                si = k.rot("sg", 2)
                k.act(sg[si][:], gp[:], AF.Sigmoid, reads=[kgp], writes=[("sg", si)])
                if n == 0:
                    k.tt("dve", m[:, dh * 512:(dh + 1) * 512], sg[si][:], up[:], ALU.mult,
                         reads=[("sg", si), kup], writes=[km])
                else:
                    pi = k.rot("pr", 2)
                    k.tt("dve", pr[pi][:], sg[si][:], up[:], ALU.mult, reads=[("sg", si), kup], writes=[("pr", pi)])
                    k.tt("pool", m[:, dh * 512:(dh + 1) * 512], m[:, dh * 512:(dh + 1) * 512], pr[pi][:], ALU.add,
                         reads=[km, ("pr", pi)], writes=[km])
        load_transposed(P, k, C, m, km, mT[b], ("mT", b), 0)
        z = zt[b]
        kz = ("zt", b)
        for dh in range(2):
            ac, kac = C["bank"]()
            for kc in range(8):
                k.mm(ac[:], mT[b][:, kc, :], wout[:, kc, dh * 512:(dh + 1) * 512], kc == 0, kc == 7,
                     reads=[("mT", b), "wout"], writes=[kac])
            k.stt(z[:, dh * 512:(dh + 1) * 512], xt[b][:, dh * 512:(dh + 1) * 512], ALPHA, ac[:],
                  ALU.mult, ALU.add, reads=[("xt", b), kac], writes=[kz])
        layer_norm_tile(P, k, z, kz, gam, bet, LN_EPS, tmp)
        P.dma("sp", dst[r0:r0 + 128, :], z[:], reads=[kz], writes=[kdst])


NIT = 16
BIGM = 1.0e9
LNS = float(2 ** 30)
SLOPES = [2.0 ** (-2 * (h + 1)) for h in range(4)]


def load_w(P, dst, src2d, key):
    P.dma("pool", dst, src2d.rearrange("(kc p) f -> p kc f", p=128), writes=[key])


def mixa1_stage(P, k, C, x1_d, kx1, o_d, ko, win_d, CD):
    P.stage_begin()
    ident = C["ident"]
    wq = P.sb([128, 8, 256], BF16)
    wk = P.sb([128, 8, 64], BF16)
    wiq = P.sb([128, 8, 128], BF16)
    wik4 = P.sb([128, 8, 128], BF16)
    wtm = P.sb([128, 8, 68], BF16)
    load_w(P, wq[:], win_d[:, 0:256], "w_a1")
    load_w(P, wk[:], win_d[:, 256:320], "w_a1")
    load_w(P, wtm[:, :, 0:64], win_d[:, 320:384], "w_a1")
    load_w(P, wiq[:, :, 0:96], win_d[:, 384:480], "w_a1")
    wiqB = P.sb([128, 8, 32], BF16)
    load_w(P, wiqB[:], win_d[:, 480:512], "w_a1")
    IQTB = P.sb([128, 512], BF16)
    for r in range(4):
        load_w(P, wik4[:, :, r * 32:(r + 1) * 32], win_d[:, 512:544], "w_a1")
    load_w(P, wtm[:, :, 64:68], win_d[:, 544:548], "w_a1")
    identb = P.sb([128, 128], BF16)
    P.dma("pool", identb[:], CD["c_ident"], writes=["identb"])
    KT = P.sb([128, SEQ], BF16)
    IKT4 = P.sb([128, SEQ], BF16)
    V = P.sb([128, 32, 64], BF16)
    P.dma("pool", KT[64:66, :], CD["c_kpos"], writes=["KTpos"])
    QA = [P.sb([128, 512], BF16) for _ in range(4)]
    for h in range(4):
        P.dma("pool", QA[h][64:66, :], CD["c_qaug"][h], writes=[("QAc", h)])
    BQ = P.sb([128, 32, 4], F32)
    P.dma("sp", BQ[:], CD["c_bq"], writes=["BQ"])
    LCORR = P.sb([128, 4, 128], F32)
    P.dma("sp", LCORR[:], CD["c_corr"], writes=["CORR"])
    QPOS = P.sb([128, 32], F32)
    P.dma("sp", QPOS[:], CD["c_qpos"], writes=["QPOS"])
    NSL = P.sb([128, 4], F32)
    P.dma("sp", NSL[:], CD["c_nsl"], writes=["NSL"])
    B4 = P.sb([128, 4], F32)
    PW2 = P.sb([128, 2, NIT], F32)
    P.dma("sp", PW2[:], CD["c_pw2"], writes=["PW2"])
    IQT = P.sb([128, 512], BF16)
    IW = [P.sb([128, 4], F32) for _ in range(4)]
    xt = [P.sb([128, D], F32) for _ in range(2)]
    xT = P.sb([128, 8, 512], BF16)
    isc = P.sb([128, SEQ], F32)
    kf = P.sb([128, SEQ], F32)
    NEGIDX = P.sb([128, SEQ], F32)
    P.dma("sp", NEGIDX[:], CD["c_negidx"], writes=["NEGIDX"])
    KPOSF = P.sb([128, SEQ], F32)
    k.ts("pool", KPOSF[:], NEGIDX[:], -4096.0, ALU.mult, reads=["NEGIDX"], writes=["KPOSF"])
    tmpk = P.sb([128, SEQ], F32)
    lgs = [P.sb([128, 512], F32) for _ in range(2)]
    msk = P.sb([128, SEQ], BF16)
    MD = P.sb([128, 4, 128], F32)
    rl = [P.sb([128, 512], F32) for _ in range(2)]
    Eb = [P.sb([128, 512], BF16) for _ in range(2)]
    Pm = [P.sb([128, 512], BF16) for _ in range(2)]
    PT = [P.sb([128, 512], BF16) for _ in range(2)]
    RS = P.sb([128, 16], F32)
    sm = P.sb([128, 16], F32)
    W2 = P.sb([128, 2, NIT], F32)
    OA = [P.sb([128, 256], F32) for _ in range(2)]
    accb = C["banks"][0:2]
    wb = C["wbank"]
    bb = C["bbank"]

    for G in range(SEQ // 512):
        c0 = G * 512
        for tt in range(4):
            b = k.rot("a1xt", 2)
            P.dma("sp", xt[b][:], x1_d[c0 + tt * 128:c0 + (tt + 1) * 128, :], reads=[kx1], writes=[("xt", b)])
            load_transposed(P, k, C, xt[b], ("xt", b), xT, "xT", tt * 128)
        for h in range(4):
            pb, kpb = wb()
            for kc in range(8):
                k.mm(pb[0:64, :], wq[:, kc, 64 * h:64 * h + 64], xT[:, kc, :], kc == 0, kc == 7,
                     reads=["xT", "w_a1"], writes=[kpb])
            k.copy("act" if h % 2 == 0 else "dve", QA[h][0:64, :], pb[0:64, :], reads=[kpb], writes=[("QA", h)])
        pb, kpb = wb()
        for kc in range(8):
            k.mm(pb[0:64, :], wk[:, kc, :], xT[:, kc, :], kc == 0, kc == 7, reads=["xT", "w_a1"], writes=[kpb])
        k.copy("dve", KT[0:64, c0:c0 + 512], pb[0:64, :], reads=[kpb], writes=[("KT", G)])
        pb, kpb = wb()
        for kc in range(8):
            k.mm(pb[:], wik4[:, kc, :], xT[:, kc, :], kc == 0, kc == 7, reads=["xT", "w_a1"], writes=[kpb])
        k.copy("act", IKT4[:, c0:c0 + 512], pb[:], reads=[kpb], writes=[("IKT", G)])
        pb, kpb = wb()
        for kc in range(8):
            k.mm(pb[0:96, :], wiq[:, kc, 0:96], xT[:, kc, :], kc == 0, kc == 7, reads=["xT", "w_a1"], writes=[kpb])
        k.copy("dve", IQT[0:96, :], pb[0:96, :], reads=[kpb], writes=["IQT"])
        pb, kpb = wb()
        for kc in range(8):
            k.mm(pb[0:32, :], wiqB[:, kc, :], xT[:, kc, :], kc == 0, kc == 7, reads=["xT", "w_a1"], writes=[kpb])
        k.copy("act", IQTB[0:32, :], pb[0:32, :], reads=[kpb], writes=["IQT"])
        for tt in range(4):
            T = 4 * G + tt
            pb, kpb = wb()
            for kc in range(8):
                k.mm(pb[:, 0:68], xT[:, kc, tt * 128:(tt + 1) * 128], wtm[:, kc, :], kc == 0, kc == 7,
                     reads=["xT", "w_a1"], writes=[kpb])
            k.copy("act", V[:, T, :], pb[:, 0:64], reads=[kpb], writes=[("V", G)])
            k.copy("dve", IW[tt][:], pb[:, 64:68], reads=[kpb], writes=[("IW", tt)])

        for tt in range(4):
            T = 4 * G + tt
            nk = 128 * (T + 1)
            NKT = (nk + 511) // 512
            kread = [("KT", g) for g in range(G + 1)] + ["KTpos"]
            ikread = [("IKT", g) for g in range(G + 1)]
            vread = [("V", g) for g in range(G + 1)]
            qs = slice(tt * 128, (tt + 1) * 128)
            for kt in range(NKT):
                wk_ = min(512, nk - 512 * kt)
                ks_ = slice(kt * 512, kt * 512 + wk_)
                for h in range(4):
                    z, kz = wb()
                    if h < 3:
                        k.mm(z[:, 0:wk_], IQT[32 * h:32 * h + 32, qs], IKT4[32 * h:32 * h + 32, ks_], True, True,
                             reads=["IQT"] + ikread, writes=[kz])
                    else:
                        k.mm(z[:, 0:wk_], IQTB[0:32, qs], IKT4[0:32, ks_], True, True,
                             reads=["IQT"] + ikread, writes=[kz])
                    ri = k.rot("rl", 2)
                    k.act(rl[ri][:, 0:wk_], z[:, 0:wk_], AF.Relu, reads=[kz], writes=[("rl", ri)])
                    if h == 0:
                        k.ts("dve", isc[:, ks_], rl[ri][:, 0:wk_], IW[tt][:, 0:1], ALU.mult,
                             reads=[("rl", ri), ("IW", tt)], writes=["isc"])
                    else:
                        k.stt(isc[:, ks_], rl[ri][:, 0:wk_], IW[tt][:, h:h + 1], isc[:, ks_], ALU.mult, ALU.add,
                              reads=[("rl", ri), ("IW", tt), "isc"], writes=["isc"])
            k.memset("pool", isc[0:64, nk - 64:nk], -BIGM, writes=["isc"])
            k.act(kf[:, 0:nk], isc[:, 0:nk], AF.Abs, reads=["isc"], writes=["kf"])
            k.act(kf[:, 0:nk], kf[:, 0:nk], AF.Ln, reads=["kf"], writes=["kf"], scale=LNS, bias=1.0)
            k.act(isc[:, 0:nk], isc[:, 0:nk], AF.Sign, reads=["isc"], writes=["isc"])
            k.tt("pool", kf[:, 0:nk], kf[:, 0:nk], isc[:, 0:nk], ALU.mult, reads=["kf", "isc"], writes=["kf"])
            k.stt(isc[:, 0:nk], kf[:, 0:nk], 0.0, NEGIDX[:, 0:nk], ALU.is_equal, ALU.mult,
                  reads=["kf", "NEGIDX", "isc"], writes=["isc"])
            k.tt("pool", kf[:, 0:nk], kf[:, 0:nk], isc[:, 0:nk], ALU.add, reads=["kf", "isc"], writes=["kf"])
            if T <= 1:
                k.memset("dve", sm[:, 6:7], -40.0, writes=["sm"])
            else:
                P.op("dve", (lambda nk: lambda e: e.reduce_max(out=sm[:, 0:1], in_=kf[:, 0:nk], axis=AX.X))(nk),
                     reads=["kf"], writes=["sm"])
                P.op("dve", (lambda nk: lambda e: e.tensor_reduce(out=sm[:, 1:2], in_=kf[:, 0:nk - 64], axis=AX.X,
                                                                   op=ALU.min))(nk), reads=["kf"], writes=["sm"])
                k.ts("dve", sm[:, 1:2], sm[:, 1:2], -1.0, ALU.add, reads=["sm"], writes=["sm"])
                k.tt("dve", sm[:, 2:3], sm[:, 0:1], sm[:, 1:2], ALU.subtract, reads=["sm"], writes=["sm"])
                k.ts("dve", W2[:].rearrange("p a b -> p (a b)"), PW2[:].rearrange("p a b -> p (a b)"), sm[:, 2:3],
                     ALU.mult, reads=["sm", "PW2"], writes=["W2"])
                k.stt(sm[:, 3:4], sm[:, 2:3], 0.5, sm[:, 1:2], ALU.mult, ALU.add, reads=["sm"], writes=["sm"])
                for it in range(NIT):
                    k.ts("dve", msk[:, 0:nk], kf[:, 0:nk], sm[:, 3:4], ALU.is_gt, reads=["kf", "sm"],
                         writes=["msk", "sm"], s2=None, op1=ALU.add, accum_out=sm[:, 4:5])
                    k.ts("dve", sm[:, 5:6], sm[:, 4:5], 255.5, ALU.is_gt, reads=["sm", "W2"], writes=["sm"],
                         s2=W2[:, 0, it:it + 1], op1=ALU.mult)
                    k.stt(sm[:, 3:4], sm[:, 5:6], W2[:, 1, it:it + 1], sm[:, 3:4], ALU.add, ALU.add,
                          reads=["sm", "W2"], writes=["sm"])
                k.tt("dve", sm[:, 6:7], sm[:, 3:4], W2[:, 1, NIT - 1:NIT], ALU.add, reads=["sm", "W2"], writes=["sm"])
            k.ts("dve", isc[:, 0:nk], kf[:, 0:nk], sm[:, 6:7], ALU.is_le, reads=["kf", "sm", "isc"], writes=["isc"],
                 s2=-30000.0, op1=ALU.mult)
            k.tt("pool", tmpk[:, 0:nk], KPOSF[:, 0:nk], isc[:, 0:nk], ALU.add, reads=["KPOSF", "isc"], writes=["tmpk"])
            P.op("dve", (lambda nk: lambda e: e.reduce_max(out=sm[:, 9:10], in_=tmpk[:, 0:nk], axis=AX.X))(nk),
                 reads=["tmpk"], writes=["sm"])
            k.ts("dve", sm[:, 9:10], sm[:, 9:10], QPOS[:, T:T + 1], ALU.min, reads=["sm", "QPOS"], writes=["sm"])
            k.ts("dve", B4[:], NSL[:], sm[:, 9:10], ALU.mult, reads=["sm", "NSL"], writes=["B4"])
            for h in range(4):
                k.tt("pool", MD[:, h, :], isc[:, nk - 128:nk], LCORR[:, h, :], ALU.add, reads=["isc", "CORR"],
                     writes=["MD"])
            oa = OA[T % 2]
            koa = ("OA", T % 2)
            for h in range(4):
                acc = accb[h % 2]
                kacc = ("ACC", h % 2)
                nslot = 0
                for kt in range(NKT):
                    wk_ = min(512, nk - 512 * kt)
                    ks_ = slice(kt * 512, kt * 512 + wk_)
                    lg, klg = wb()
                    k.mm(lg[:, 0:wk_], QA[h][0:66, qs], KT[0:66, ks_], True, True,
                         reads=[("QA", h), ("QAc", h)] + kread, writes=[klg])
                    last = kt == NKT - 1
                    wpl = wk_ - 128 if last else wk_
                    li = k.rot("lgs", 2)
                    if wpl > 0:
                        k.stt(lgs[li][:, 0:wpl], lg[:, 0:wpl], 0.125, isc[:, kt * 512:kt * 512 + wpl], ALU.mult,
                              ALU.add, reads=[klg, "isc"], writes=[("lgs", li)])
                    if last:
                        k.stt(lgs[li][:, wpl:wk_], lg[:, wpl:wk_], 0.125, MD[:, h, :], ALU.mult, ALU.add,
                              reads=[klg, "MD"], writes=[("lgs", li)])
                    pi = k.rot("Pm", 2)
                    k.act(Pm[pi][:, 0:wk_], lgs[li][:, 0:wk_], AF.Exp, reads=[("lgs", li), "B4"],
                          writes=[("Pm", pi), "RS"], bias=B4[:, h:h + 1], accum_out=RS[:, nslot:nslot + 1])
                    nslot += 1
                    nb = wk_ // 128
                    tp, ktp = bb()
                    for j in range(nb):
                        k.tr(tp[:, j * 128:(j + 1) * 128], Pm[pi][:, j * 128:(j + 1) * 128], identb[:],
                             reads=[("Pm", pi), "identb"], writes=[ktp])
                    ti = k.rot("PT", 2)
                    k.copy("act" if kt % 2 == 0 else "dve", PT[ti][:, 0:wk_], tp[:, 0:wk_], reads=[ktp],
                           writes=[("PT", ti)])
                    for j in range(nb):
                        k.mm(acc[:, 0:64], PT[ti][:, j * 128:(j + 1) * 128], V[:, kt * 4 + j, :],
                             kt == 0 and j == 0, last and j == nb - 1, reads=[("PT", ti)] + vread, writes=[kacc])
                P.op("dve", (lambda n: lambda e: e.reduce_sum(out=sm[:, 7:8], in_=RS[:, 0:n], axis=AX.X))(nslot),
                     reads=["RS"], writes=["sm"])
                P.op("dve", lambda e: e.reciprocal(out=sm[:, 8:9], in_=sm[:, 7:8]), reads=["sm"], writes=["sm"])
                k.ts("dve", oa[:, 64 * h:64 * h + 64], acc[:, 0:64], sm[:, 8:9], ALU.mult, reads=[kacc, "sm"],
                     writes=[koa])
            P.dma("sp", o_d[T * 128:(T + 1) * 128, 0:256], oa[:], reads=[koa], writes=[ko])


def small_rstd(P, k, ss, key, scale, eps):
    k.ts("dve", ss, ss, scale, ALU.mult, reads=[key], writes=[key], s2=eps, op1=ALU.add)
    k.act(ss, ss, AF.Sqrt, reads=[key], writes=[key])
    P.op("dve", lambda e: e.reciprocal(out=ss, in_=ss), reads=[key], writes=[key])


def rope_ops(P, k, x1, x2, cosb, sinb, o1, o2, tmps, kin, kout, ktmp):
    t1, t2, t3, t4 = tmps
    k.tt("dve", t1, cosb, x1, ALU.mult, reads=[kin, "rope"], writes=[ktmp])
    k.tt("dve", t2, sinb, x2, ALU.mult, reads=[kin, "rope"], writes=[ktmp])
    k.tt("pool", o1, t1, t2, ALU.subtract, reads=[ktmp], writes=[kout])
    k.tt("dve", t3, sinb, x1, ALU.mult, reads=[kin, "rope"], writes=[ktmp])
    k.tt("dve", t4, cosb, x2, ALU.mult, reads=[kin, "rope"], writes=[ktmp])
    k.tt("pool", o2, t3, t4, ALU.add, reads=[ktmp], writes=[kout])


def mixa2_stage(P, k, C, x1_d, kx1, o_d, ko, win_d, CD, gn_d, qn_d, kvn_d, wuq_d, wukv_d, do_ret=True, do_mla=True):
    P.stage_begin()
    banks = C["banks"]
    wc2 = [0]

    def wb():
        i = 4 + wc2[0] % 2
        wc2[0] += 1
        return banks[i], ("PW", i - 4)
    bb = C["bbank"]
    wrq = P.sb([128, 8, 256], BF16)
    wrk = P.sb([128, 8, 256], BF16)
    wtm = P.sb([128, 8, 1184], BF16)
    load_w(P, wrq[:], win_d[:, 548:804], "w_a2")
    load_w(P, wrk[:], win_d[:, 804:1060], "w_a2")
    load_w(P, wtm[:, :, 0:768], win_d[:, 804:1572], "w_a2")
    load_w(P, wtm[:, :, 768:1184], win_d[:, 1828:2244], "w_a2")
    identb = P.sb([128, 128], BF16)
    P.dma("pool", identb[:], CD["c_ident"], writes=["identb"])
    wuq_f = P.sb([128, 2, 384], F32)
    wukv_f = P.sb([128, 512], F32)
    qn = P.sb([128, 2], F32)
    kvn = P.sb([128, 1], F32)
    wuq = P.sb([128, 2, 384], BF16)
    wukv = P.sb([128, 512], BF16)
    P.dma("sp", wuq_f[:], wuq_d.rearrange("(c p) f -> p c f", p=128), writes=["wuq_f"])
    P.dma("sp", wukv_f[:], wukv_d, writes=["wukv_f"])
    for c2 in range(2):
        P.dma("sp", qn[:, c2:c2 + 1], qn_d[c2 * 128:(c2 + 1) * 128].rearrange("(p c) -> p c", c=1), writes=["qn"])
    P.dma("sp", kvn[:], kvn_d.rearrange("(p c) -> p c", c=1), writes=["kvn"])
    for c2 in range(2):
        k.ts("dve", wuq[:, c2, :], wuq_f[:, c2, :], qn[:, c2:c2 + 1], ALU.mult, reads=["wuq_f", "qn"], writes=["wuq"])
    k.ts("dve", wukv[:], wukv_f[:], kvn[:, 0:1], ALU.mult, reads=["wukv_f", "kvn"], writes=["wukv"])
    XI = P.sb([128, 2, 512], F32)
    P.dma("sp", XI[:], CD["c_xi"], writes=["XI"])
    ZETA = P.sb([128, 256], F32)
    P.dma("sp", ZETA[:], CD["c_zeta"], writes=["ZETA"])
    DECT = P.sb([128, 512], F32)
    P.dma("sp", DECT[:], CD["c_dect"], writes=["DECT"])
    GCH = P.sb([128, 128], F32)
    P.dma("sp", GCH[:], CD["c_gch"], writes=["GCH"])
    CMT = P.sb([128, 512], BF16)
    P.dma("pool", CMT[:], CD["c_cmt"], writes=["CMT"])
    COS = P.sb([128, 32, 16], F32)
    SIN = P.sb([128, 32, 16], F32)
    P.dma("sp", COS[:], CD["c_cos"], writes=["rope"])
    P.dma("sp", SIN[:], CD["c_sin"], writes=["rope"])
    GNW = P.sb([128, 256], F32)
    P.dma("sp", GNW[:], bcast_rows(gn_d, 256), writes=["GNW"])
    KTm = P.sb([128, 4, SEQ], BF16)
    Vm = P.sb([128, 32, 4, 65], BF16)
    k.memset("pool", Vm[:].rearrange("p a b c -> p (a b c)"), 1.0, writes=["Vm1"])
    state = P.sb([128, 128], F32)
    state_b = P.sb([128, 128], BF16)
    k.memset("dve", state[:], 0.0, writes=["state"])
    k.memset("dve", state_b[:], 0.0, writes=["state_b"])
    xt = [P.sb([128, D], F32) for _ in range(2)]
    xT = P.sb([128, 8, 512], BF16)
    rqT = P.sb([128, 2, 512], BF16)
    rqxTz = [P.sb([128, 512], BF16) for _ in range(4)]
    rkTz = [P.sb([128, 512], BF16) for _ in range(4)]
    for h in range(4):
        k.memset("pool", rqxTz[h][:], 0.0, writes=["rqxTz"])
        k.memset("pool", rkTz[h][:], 0.0, writes=["rkTz"])
    rkz = [P.sb([128, 256], BF16) for _ in range(4)]
    rv = [P.sb([128, 256], BF16) for _ in range(4)]
    rgs = [P.sb([128, 256], F32) for _ in range(4)]
    cqn = [P.sb([128, 256], BF16) for _ in range(4)]
    ckvn = [P.sb([128, 128], BF16) for _ in range(4)]
    krs = [P.sb([128, 32], F32) for _ in range(4)]
    junk = P.sb([128, 256], F32)
    cqf = P.sb([128, 256], F32)
    qf = P.sb([128, 384], F32)
    kvf = P.sb([128, 512], F32)
    accs = P.sb([128, 4, 65], F32)
    ss = P.sb([128, 8], F32)
    PTr = P.sb([128, 512], BF16)
    st6 = P.sb([128, 4, 6], F32)
    mv4 = P.sb([128, 4, 2], F32)
    rs4 = P.sb([128, 4], F32)
    retn = P.sb([128, 256], F32)
    cqnT = P.sb([128, 2, 128], BF16)
    qtm = P.sb([128, 384], BF16)
    qT = P.sb([128, 4, 128], BF16)
    ckvnT = P.sb([128, 128], BF16)
    ktm = P.sb([128, 4, 96], BF16)
    krr = P.sb([128, 32], F32)
    rt = [P.sb([128, 64], F32) for _ in range(4)]
    PTm = [P.sb([128, 512], BF16) for _ in range(2)]
    od = [P.sb([128, 768], F32) for _ in range(2)]
    k.memset("pool", od[0][:], 0.0, writes=[("od", 0)])
    k.memset("pool", od[1][:], 0.0, writes=[("od", 1)])
    SC = 96.0 ** -0.5
    import os
    NGDBG = int(os.environ.get("A2_MAXG", SEQ // 512))
    if os.environ.get("A2_VAR", "") == "B":
        P.barrier()
    CUT = int(os.environ.get("A2_CUT", 9))
    CUTP = int(os.environ.get("A2_CUTP", 9))

    for G in range(NGDBG):
        c0 = G * 512
        for tt in range(4):
            b = k.rot("a2xt", 2)
            P.dma("sp", xt[b][:], x1_d[c0 + tt * 128:c0 + (tt + 1) * 128, :], reads=[kx1], writes=[("xt", b)])
            load_transposed(P, k, C, xt[b], ("xt", b), xT, "xT", tt * 128)
        if do_ret and CUTP >= 1:
            for t in range(2):
                pb, kpb = wb()
                for kc in range(8):
                    k.mm(pb[:], wrq[:, kc, t * 128:(t + 1) * 128], xT[:, kc, :], kc == 0, kc == 7,
                         reads=["xT", "w_a2"], writes=[kpb])
                k.copy("act", rqT[:, t, :], pb[:], reads=[kpb], writes=["rqT"])
                for hh in range(2 if CUTP >= 2 else 0):
                    rs_ = slice(64 * hh, 64 * hh + 64)
                    k.tt("dve", rqxTz[2 * t + hh][rs_, :], XI[rs_, t, :], pb[rs_, :], ALU.mult, reads=[kpb, "XI"],
                         writes=["rqxTz"])
                if CUTP < 3:
                    continue
                pb, kpb = wb()
                for kc in range(8):
                    k.mm(pb[:], wrk[:, kc, t * 128:(t + 1) * 128], xT[:, kc, :], kc == 0, kc == 7,
                         reads=["xT", "w_a2"], writes=[kpb])
                for hh in range(2):
                    rs_ = slice(64 * hh, 64 * hh + 64)
                    k.copy("act" if hh == 0 else "dve", rkTz[2 * t + hh][rs_, :], pb[rs_, :], reads=[kpb], writes=["rkTz"])
        for tt in range(4):
            ts_ = slice(tt * 128, (tt + 1) * 128)
            if do_ret:
                pb, kpb = wb()
                for kc in range(8):
                    k.mm(pb[:], xT[:, kc, ts_], wtm[:, kc, 0:512], kc == 0, kc == 7, reads=["xT", "w_a2"], writes=[kpb])
                if os.environ.get("A2_VAR", "") == "A":
                    k.copy("act", rkz[tt][:], pb[:, 0:256], reads=[kpb], writes=[("rkz", tt)])
                else:
                    k.tt("dve", rkz[tt][:], ZETA[:], pb[:, 0:256], ALU.mult, reads=[kpb, "ZETA"], writes=[("rkz", tt)])
                k.copy("act", rv[tt][:], pb[:, 256:512], reads=[kpb], writes=[("rv", tt)])
            pb, kpb = wb()
            for kc in range(8):
                k.mm(pb[:], xT[:, kc, ts_], wtm[:, kc, 512:1024], kc == 0, kc == 7, reads=["xT", "w_a2"], writes=[kpb])
            k.act(rgs[tt][:], pb[:, 0:256], AF.Silu, reads=[kpb], writes=[("rgs", tt)])
            if do_mla:
                k.act(junk[:], pb[:, 256:512], AF.Square, reads=[kpb], writes=["junk", "ss"], accum_out=ss[:, 0:1])
                small_rstd(P, k, ss[:, 0:1], "ss", 1.0 / 256.0, RMS_EPS)
                k.copy("act", cqf[:], pb[:, 256:512], reads=[kpb], writes=["cqf"])
                k.ts("dve", cqn[tt][:], cqf[:], ss[:, 0:1], ALU.mult, reads=["cqf", "ss"], writes=[("cqn", tt)])
                pb, kpb = wb()
                for kc in range(8):
                    k.mm(pb[:, 0:160], xT[:, kc, ts_], wtm[:, kc, 1024:1184], kc == 0, kc == 7,
                         reads=["xT", "w_a2"], writes=[kpb])
                k.act(junk[:, 0:128], pb[:, 0:128], AF.Square, reads=[kpb], writes=["junk", "ss"], accum_out=ss[:, 1:2])
                small_rstd(P, k, ss[:, 1:2], "ss", 1.0 / 128.0, RMS_EPS)
                k.copy("act", cqf[:, 0:128], pb[:, 0:128], reads=[kpb], writes=["cqf"])
                k.ts("dve", ckvn[tt][:], cqf[:, 0:128], ss[:, 1:2], ALU.mult, reads=["cqf", "ss"], writes=[("ckvn", tt)])
                k.copy("act", krs[tt][:], pb[:, 128:160], reads=[kpb], writes=[("krs", tt)])

        for tt in range(4):
            T = 4 * G + tt
            q0 = T * 128
            o = od[T % 2]
            kod = ("od", T % 2)
            if do_ret and CUT >= 1:
                O = banks[2]
                kO = ("ACC", 2)
                KV = banks[3]
                kKV = ("ACC", 3)
                ns_ = slice(tt * 128, (tt + 1) * 128)
                sT, ksT = wb()
                for h in range(4):
                    k.mm(sT[:, 128 * h:128 * h + 128], rkTz[h][:, ns_], rqT[:, h // 2, ns_], True, True,
                         reads=["rkTz", "rqT"], writes=[ksT])
                k.tt("dve", PTr[:], DECT[:], sT[:], ALU.mult, reads=[ksT, "DECT"], writes=["PTr"])
                for h in range(4 if CUT >= 2 else 0):
                    t = h // 2
                    k.mm(O[:, 64 * h:64 * h + 64], PTr[:, 128 * h:128 * h + 128], rv[tt][:, 64 * h:64 * h + 64],
                         True, False, reads=["PTr", ("rv", tt)], writes=[kO])
                    k.mm(O[:, 64 * h:64 * h + 64], rqxTz[h][:, ns_], state_b[:, 64 * t:64 * t + 64],
                         False, True, reads=["rqxTz", "state_b"], writes=[kO])
                for h in range(4 if CUT >= 3 else 0):
                    t = h // 2
                    k.mm(KV[:, 64 * h:64 * h + 64], rkz[tt][:, 128 * t:128 * t + 128], rv[tt][:, 64 * h:64 * h + 64],
                         True, True, reads=[("rkz", tt), ("rv", tt)], writes=[kKV])
                k.tt("pool", state[:], state[:], GCH[:], ALU.mult, reads=["state", "GCH"], writes=["state"])
                for h in range(4 if CUT >= 3 else 0):
                    hb = 64 * (h % 2)
                    t = h // 2
                    k.tt("dve", state[hb:hb + 64, 64 * t:64 * t + 64], state[hb:hb + 64, 64 * t:64 * t + 64],
                         KV[hb:hb + 64, 64 * h:64 * h + 64], ALU.add, reads=["state", kKV], writes=["state"])
                k.copy("act", state_b[:], state[:], reads=["state"], writes=["state_b"])
                for h in range(4 if CUT >= 4 else 0):
                    P.op("dve", (lambda h: lambda e: e.bn_stats(out=st6[:, h, :], in_=O[:, 64 * h:64 * h + 64]))(h),
                         reads=[kO], writes=["st6"])
                    P.op("dve", (lambda h: lambda e: e.bn_aggr(out=mv4[:, h, :], in_=st6[:, h, :]))(h),
                         reads=["st6"], writes=["mv4"])
                if CUT >= 4:
                    k.ts("dve", rs4[:], mv4[:, :, 1], LN_EPS, ALU.add, reads=["mv4"], writes=["rs4"])
                    k.act(rs4[:], rs4[:], AF.Sqrt, reads=["rs4"], writes=["rs4"])
                    P.op("dve", lambda e: e.reciprocal(out=rs4[:], in_=rs4[:]), reads=["rs4"], writes=["rs4"])
                for h in range(4 if CUT >= 4 else 0):
                    k.ts("dve", retn[:, 64 * h:64 * h + 64], O[:, 64 * h:64 * h + 64], mv4[:, h, 0:1], ALU.subtract,
                         reads=[kO, "mv4", "rs4"], writes=["retn"], s2=rs4[:, h:h + 1], op1=ALU.mult)
                k.tt("pool", retn[:], retn[:], GNW[:], ALU.mult, reads=["retn", "GNW"], writes=["retn"])
                k.tt("pool", o[:, 0:256], retn[:], rgs[tt][:], ALU.mult, reads=["retn", ("rgs", tt)], writes=[kod])
            if do_mla:
                tp, ktp = bb()
                for c2 in range(2):
                    k.tr(tp[:, c2 * 128:(c2 + 1) * 128], cqn[tt][:, c2 * 128:(c2 + 1) * 128], identb[:],
                         reads=[("cqn", tt), "identb"], writes=[ktp])
                k.copy("act", cqnT[:].rearrange("p a b -> p (a b)"), tp[:, 0:256], reads=[ktp], writes=["cqnT"])
                qp, kqp = wb()
                for c2 in range(2):
                    k.mm(qp[:, 0:384], cqnT[:, c2, :], wuq[:, c2, :], c2 == 0, c2 == 1, reads=["cqnT", "wuq"], writes=[kqp])
                k.copy("act", qf[:], qp[:, 0:384], reads=[kqp], writes=["qf"])
                kqp = "qf"
                qv = qf[:].rearrange("p (h c) -> p h c", c=96)
                qo = qtm[:].rearrange("p (h c) -> p h c", c=96)
                k.copy("pool", qo[:, :, 0:64], qv[:, :, 0:64], reads=[kqp], writes=["qtm"])
                cosb = COS[:, T:T + 1, :].to_broadcast([128, 4, 16])
                sinb = SIN[:, T:T + 1, :].to_broadcast([128, 4, 16])
                tm = [r_[:].rearrange("p (h c) -> p h c", c=16) for r_ in rt]
                rope_ops(P, k, qv[:, :, 64:80], qv[:, :, 80:96], cosb, sinb, qo[:, :, 64:80], qo[:, :, 80:96], tm,
                         kqp, "qtm", "rt")
                tp, ktp = bb()
                for h in range(4):
                    k.tr(tp[0:96, h * 128:(h + 1) * 128], qtm[:, 96 * h:96 * h + 96], identb[:],
                         reads=["qtm", "identb"], writes=[ktp])
                k.copy("dve", qT[0:96].rearrange("p a b -> p (a b)"), tp[0:96, 0:512], reads=[ktp], writes=["qT"])
                tp, ktp = bb()
                k.tr(tp[:, 0:128], ckvn[tt][:], identb[:], reads=[("ckvn", tt), "identb"], writes=[ktp])
                k.copy("act", ckvnT[:], tp[:, 0:128], reads=[ktp], writes=["ckvnT"])
                kp, kkp = wb()
                k.mm(kp[:], ckvnT[:], wukv[:], True, True, reads=["ckvnT", "wukv"], writes=[kkp])
                k.copy("act", kvf[:], kp[:], reads=[kkp], writes=["kvf"])
                kkp = "kvf"
                kvv = kvf[:].rearrange("p (h c) -> p h c", c=128)
                k.copy("pool", Vm[:, T, :, 0:64], kvv[:, :, 64:128], reads=[kkp, "Vm1"], writes=[("Vm", G)])
                k.copy("dve", ktm[:, :, 0:64], kvv[:, :, 0:64], reads=[kkp], writes=["ktm"])
                c1 = COS[:, T, :]
                s1 = SIN[:, T, :]
                rope_ops(P, k, krs[tt][:, 0:16], krs[tt][:, 16:32], c1, s1, krr[:, 0:16], krr[:, 16:32],
                         [r_[:, 0:16] for r_ in rt], ("krs", tt), "krr", "rt")
                k.copy("pool", ktm[:, :, 64:96], krr[:].unsqueeze(1).to_broadcast([128, 4, 32]), reads=["krr"],
                       writes=["ktm"])
                tp, ktp = bb()
                for h in range(4):
                    k.tr(tp[0:96, h * 128:(h + 1) * 128], ktm[:, h, :], identb[:], reads=["ktm", "identb"], writes=[ktp])
                k.copy("dve", KTm[0:96, :, q0:q0 + 128], tp[0:96, 0:512].rearrange("p (h c) -> p h c", c=128),
                       reads=[ktp], writes=[("KTm", G)])
                ktread = [("KTm", g) for g in range(G + 1)]
                vmread = [("Vm", g) for g in range(G + 1)] + ["Vm1"]
                for j in range(T + 1):
                    stp, kst = wb()
                    for h in range(4):
                        k.mm(stp[:, h * 128:(h + 1) * 128], KTm[0:96, h, j * 128:(j + 1) * 128], qT[0:96, h, :], True, True,
                             reads=["qT"] + ktread, writes=[kst])
                    pi = k.rot("PTm", 2)
                    k.act(PTm[pi][:], stp[:], AF.Exp, reads=[kst], writes=[("PTm", pi)], scale=SC)
                    if j == T:
                        k.tt("pool", PTm[pi][:], PTm[pi][:], CMT[:], ALU.mult, reads=[("PTm", pi), "CMT"],
                             writes=[("PTm", pi)])
                    for h in range(4):
                        k.mm(banks[h][:, 0:65], PTm[pi][:, h * 128:(h + 1) * 128], Vm[:, j, h, :], j == 0, j == T,
                             reads=[("PTm", pi)] + vmread, writes=[("ACC", h)])
                for h in range(4):
                    k.copy("act", accs[:, h, :], banks[h][:, 0:65], reads=[("ACC", h)], writes=["accs"])
                for h in range(4):
                    P.op("dve", (lambda h: lambda e: e.reciprocal(out=ss[:, 4 + h:5 + h], in_=accs[:, h, 64:65]))(h),
                         reads=["accs"], writes=["ss"])
                    k.ts("dve", o[:, 512 + 64 * h:512 + 64 * h + 64], accs[:, h, 0:64], ss[:, 4 + h:5 + h], ALU.mult,
                         reads=["accs", "ss"], writes=[kod])
            P.dma("sp", o_d[q0:q0 + 128, 256:512], o[:, 0:256], reads=[kod], writes=[ko])
            P.dma("sp", o_d[q0:q0 + 128, 768:1024], o[:, 512:768], reads=[kod], writes=[ko])


TWO_PI = 2.0 * math.pi


def sincos(P, k, ang, shape, out_s, out_c, T, key_in, key_out):
    r, ri, rf, m = T["r"], T["ri"], T["rf"], T["m"]
    sl = tuple(slice(0, n) for n in shape)
    for off, out in ((0.5, out_s), (0.75, out_c)):
        k.ts("dve", r[sl], ang, 1.0 / TWO_PI, ALU.mult, reads=[key_in], writes=["sc_t"], s2=off, op1=ALU.add)
        k.copy("dve", ri[sl], r[sl], reads=["sc_t"], writes=["sc_t"])
        k.copy("dve", rf[sl], ri[sl], reads=["sc_t"], writes=["sc_t"])
        k.tt("dve", r[sl], r[sl], rf[sl], ALU.subtract, reads=["sc_t"], writes=["sc_t"])
        k.ts("dve", m[sl], r[sl], 0.0, ALU.is_lt, reads=["sc_t"], writes=["sc_t"])
        k.tt("dve", r[sl], r[sl], m[sl], ALU.add, reads=["sc_t"], writes=["sc_t"])
        k.ts("dve", m[sl], r[sl], 1.0, ALU.is_ge, reads=["sc_t"], writes=["sc_t"])
        k.tt("dve", r[sl], r[sl], m[sl], ALU.subtract, reads=["sc_t"], writes=["sc_t"])
        k.ts("dve", r[sl], r[sl], TWO_PI, ALU.mult, reads=["sc_t"], writes=["sc_t"], s2=-math.pi, op1=ALU.add)
        k.act(out, r[sl], AF.Sin, reads=["sc_t"], writes=[key_out])


def mixa3_stage(P, k, C, x1_d, kx1, o_d, ko, win_d, CD, S5D, wglu_d):
    P.stage_begin()
    banks = C["banks"]
    wc2 = [0]

    def wb():
        i = 4 + wc2[0] % 2
        wc2[0] += 1
        return banks[i], ("PW", i - 4)
    wsu = P.sb([128, 8, 256], BF16)
    load_w(P, wsu[:], win_d[:, 1572:1828], "w_a3")
    wglu = P.sb([128, 2, 512], BF16)
    P.dma("pool", wglu[:], wglu_d.rearrange("(c p) f -> p c f", p=128), writes=["wglu"])

    def ld(name, shape):
        t = P.sb(shape, F32)
        P.dma("sp", t[:], S5D[name], writes=[name])
        return t
    c_are = ld("s5c_are", [128, 2, 64])
    c_aim = ld("s5c_aim", [128, 2, 64])
    c_bre = ld("s5c_bre", [128, 2, 64])
    c_bim = ld("s5c_bim", [128, 2, 64])
    c_ls = ld("s5c_ls", [128, 2])
    c_d = ld("s5c_d", [128, 2])
    s_are = ld("s5s_are", [128, 8])
    s_aim = ld("s5s_aim", [128, 8])
    s_ls = ld("s5s_ls", [128, 8])
    s_cre = ld("s5s_cre", [128, 8, 16])
    s_cim = ld("s5s_cim", [128, 8, 16])
    MSK = P.sb([128, 2], F32)
    P.dma("sp", MSK[:], CD["c_msk2"], writes=["MSK"])
    RM = P.sb([128, 4], F32)
    P.dma("sp", RM[:], CD["c_rm"], writes=["RM"])
    IOTA = P.sb([128, 256], F32)
    P.dma("sp", IOTA[:], CD["c_iota"], writes=["IOTA"])
    T = {"r": P.sb([128, 256], F32), "ri": P.sb([128, 256], I32), "rf": P.sb([128, 256], F32),
         "m": P.sb([128, 256], F32)}

    def f32(shape):
        return P.sb(shape, F32)
    dtc = f32([128, 2])
    k.act(dtc[:], c_ls[:], AF.Exp, reads=["s5c_ls"], writes=["dtc"])
    ar, ai, mag, sn, cs = f32([128, 128]), f32([128, 128]), f32([128, 128]), f32([128, 128]), f32([128, 128])
    are2 = c_are[:].rearrange("p a b -> p (a b)")
    aim2 = c_aim[:].rearrange("p a b -> p (a b)")
    bre2 = c_bre[:].rearrange("p a b -> p (a b)")
    bim2 = c_bim[:].rearrange("p a b -> p (a b)")
    for c2 in range(2):
        cs_ = slice(64 * c2, 64 * c2 + 64)
        k.ts("dve", ar[:, cs_], c_are[:, c2, :], dtc[:, c2:c2 + 1], ALU.mult, reads=["s5c_are", "dtc"], writes=["ar"])
        k.ts("dve", ai[:, cs_], c_aim[:, c2, :], dtc[:, c2:c2 + 1], ALU.mult, reads=["s5c_aim", "dtc"], writes=["ai"])
    k.act(mag[:], ar[:], AF.Exp, reads=["ar"], writes=["mag"])
    sincos(P, k, ai[:], [128, 128], sn[:], cs[:], T, "ai", "sncs")
    abr, abi, nr, den, t0, t1, cr, ci = [f32([128, 128]) for _ in range(8)]
    k.tt("dve", abr[:], mag[:], cs[:], ALU.mult, reads=["mag", "sncs"], writes=["abr"])
    k.tt("dve", abi[:], mag[:], sn[:], ALU.mult, reads=["mag", "sncs"], writes=["abi"])
    k.ts("dve", nr[:], abr[:], -1.0, ALU.add, reads=["abr"], writes=["nr"])
    k.tt("dve", den[:], are2, are2, ALU.mult, reads=["s5c_are"], writes=["den"])
    k.tt("dve", t0[:], aim2, aim2, ALU.mult, reads=["s5c_aim"], writes=["t0"])
    k.tt("dve", den[:], den[:], t0[:], ALU.add, reads=["den", "t0"], writes=["den"])
    P.op("dve", lambda e: e.reciprocal(out=den[:], in_=den[:]), reads=["den"], writes=["den"])
    k.tt("dve", t0[:], nr[:], are2, ALU.mult, reads=["nr", "s5c_are"], writes=["t0"])
    k.tt("dve", t1[:], abi[:], aim2, ALU.mult, reads=["abi", "s5c_aim"], writes=["t1"])
    k.tt("dve", t0[:], t0[:], t1[:], ALU.add, reads=["t0", "t1"], writes=["t0"])
    k.tt("dve", cr[:], t0[:], den[:], ALU.mult, reads=["t0", "den"], writes=["cr"])
    k.tt("dve", t0[:], abi[:], are2, ALU.mult, reads=["abi", "s5c_are"], writes=["t0"])
    k.tt("dve", t1[:], nr[:], aim2, ALU.mult, reads=["nr", "s5c_aim"], writes=["t1"])
    k.tt("dve", t0[:], t0[:], t1[:], ALU.subtract, reads=["t0", "t1"], writes=["t0"])
    k.tt("dve", ci[:], t0[:], den[:], ALU.mult, reads=["t0", "den"], writes=["ci"])
    Bre, Bim = f32([128, 128]), f32([128, 128])
    k.tt("dve", t0[:], cr[:], bre2, ALU.mult, reads=["cr", "s5c_bre"], writes=["t0"])
    k.tt("dve", t1[:], ci[:], bim2, ALU.mult, reads=["ci", "s5c_bim"], writes=["t1"])
    k.tt("dve", Bre[:], t0[:], t1[:], ALU.subtract, reads=["t0", "t1"], writes=["Bre"])
    k.tt("dve", t0[:], cr[:], bim2, ALU.mult, reads=["cr", "s5c_bim"], writes=["t0"])
    k.tt("dve", t1[:], ci[:], bre2, ALU.mult, reads=["ci", "s5c_bre"], writes=["t1"])
    k.tt("dve", Bim[:], t0[:], t1[:], ALU.add, reads=["t0", "t1"], writes=["Bim"])
    Bfull = f32([128, 2, 2, 128])
    for c2 in range(2):
        for ri_, src in ((0, Bre), (1, Bim)):
            for hf in range(2):
                k.ts("dve", Bfull[:, c2, ri_, 64 * hf:64 * hf + 64], src[:, 64 * c2:64 * c2 + 64], MSK[:, hf:hf + 1],
                     ALU.mult, reads=["Bre", "Bim", "MSK"], writes=["Bfull"])
    BW = P.sb([128, 8, 2, 128], BF16)
    for i in range(8):
        for ri_ in range(2):
            k.ts("dve", BW[:, i, ri_, :], Bfull[:, i // 4, ri_, :], RM[:, i % 4:i % 4 + 1], ALU.mult,
                 reads=["Bfull", "RM"], writes=["BW"])
    dts, mags, th = f32([128, 8]), f32([128, 8]), f32([128, 8])
    k.act(dts[:], s_ls[:], AF.Exp, reads=["s5s_ls"], writes=["dts"])
    k.tt("dve", mags[:], s_are[:], dts[:], ALU.mult, reads=["s5s_are", "dts"], writes=["mags"])
    k.act(mags[:], mags[:], AF.Exp, reads=["mags"], writes=["mags"])
    k.tt("dve", th[:], s_aim[:], dts[:], ALU.mult, reads=["s5s_aim", "dts"], writes=["th"])
    SINT = P.sb([128, 8, 256], F32)
    COST = P.sb([128, 8, 256], F32)
    angt = f32([128, 256])
    for i in range(8):
        k.ts("dve", angt[:], IOTA[:], th[:, i:i + 1], ALU.mult, reads=["IOTA", "th"], writes=["angt"])
        sincos(P, k, angt[:], [128, 256], SINT[:, i, :], COST[:, i, :], T, "angt", "tabs")
    ROTS, ROTC, a256 = f32([128, 8]), f32([128, 8]), f32([128, 8])
    k.ts("dve", a256[:], th[:], 256.0, ALU.mult, reads=["th"], writes=["a256"])
    sincos(P, k, a256[:], [128, 8], ROTS[:], ROTC[:], T, "a256", "rot")
    CW = P.sb([128, 8, 4, 128], BF16)
    k.memset("pool", CW[:].rearrange("p a b c -> p (a b c)"), 0.0, writes=["CW"])
    for i in range(8):
        r_ = i % 4
        for hf in range(2):
            ps_ = slice(64 * hf, 64 * hf + 64)
            cols = slice(32 * r_ + 16 * hf, 32 * r_ + 16 * hf + 16)
            k.copy("dve", CW[ps_, i, 0, cols], s_cre[ps_, i, :], reads=["s5s_cre", "CW"], writes=["CW"])
            k.ts("dve", CW[ps_, i, 1, cols], s_cre[ps_, i, :], -1.0, ALU.mult, reads=["s5s_cre", "CW"], writes=["CW"])
            k.ts("dve", CW[ps_, i, 2, cols], s_cim[ps_, i, :], -1.0, ALU.mult, reads=["s5s_cim", "CW"], writes=["CW"])
            k.ts("dve", CW[ps_, i, 3, cols], s_cim[ps_, i, :], -1.0, ALU.mult, reads=["s5s_cim", "CW"], writes=["CW"])
    qi_re, qi_im = f32([128, 8]), f32([128, 8])
    k.memset("dve", qi_re[:], 0.0, writes=["qi"])
    k.memset("dve", qi_im[:], 0.0, writes=["qi"])
    hd = f32([128, 4])
    xt = [P.sb([128, D], F32) for _ in range(2)]
    xT = P.sb([128, 8, 512], BF16)
    uT = P.sb([128, 2, 512], BF16)
    du = f32([128, 2, 512])
    tA, tB, mre, mim, qre, qim = [f32([128, 256]) for _ in range(6)]
    PV = [P.sb([128, 256], BF16) for _ in range(4)]
    yb, y2, sg = f32([128, 256]), f32([128, 256]), f32([128, 256])
    gT = P.sb([128, 2, 256], BF16)
    sgt = f32([128, 256])
    OC = [f32([128, 256]) for _ in range(2)]

    for G in range(SEQ // 512):
        c0 = G * 512
        for tt in range(4):
            b = k.rot("a3xt", 2)
            P.dma("sp", xt[b][:], x1_d[c0 + tt * 128:c0 + (tt + 1) * 128, :], reads=[kx1], writes=[("xt", b)])
            load_transposed(P, k, C, xt[b], ("xt", b), xT, "xT", tt * 128)
        for c2 in range(2):
            pb, kpb = wb()
            for kc in range(8):
                k.mm(pb[:], wsu[:, kc, c2 * 128:(c2 + 1) * 128], xT[:, kc, :], kc == 0, kc == 7,
                     reads=["xT", "w_a3"], writes=[kpb])
            k.copy("act", uT[:, c2, :], pb[:], reads=[kpb], writes=["uT"])
            k.ts("dve", du[:, c2, :], pb[:], c_d[:, c2:c2 + 1], ALU.mult, reads=[kpb, "s5c_d"], writes=["du"])
        for sb_ in range(2):
            cs_ = slice(sb_ * 256, (sb_ + 1) * 256)
            for i in range(8):
                c2 = i // 4
                bu, kbu = wb()
                for ri_ in range(2):
                    k.mm(bu[:, 256 * ri_:256 * ri_ + 256], BW[:, i, ri_, :], uT[:, c2, cs_], True, True,
                         reads=["BW", "uT"], writes=[kbu])
                Ct = COST[:, i, :]
                St = SINT[:, i, :]
                k.tt("dve", tA[:], Ct, bu[:, 0:256], ALU.mult, reads=["tabs", kbu], writes=["tA"])
                k.tt("dve", tB[:], St, bu[:, 256:512], ALU.mult, reads=["tabs", kbu], writes=["tB"])
                k.tt("pool", mre[:], tA[:], tB[:], ALU.add, reads=["tA", "tB"], writes=["mre"])
                k.tt("dve", tA[:], Ct, bu[:, 256:512], ALU.mult, reads=["tabs", kbu, "tA"], writes=["tA"])
                k.tt("dve", tB[:], St, bu[:, 0:256], ALU.mult, reads=["tabs", kbu, "tB"], writes=["tB"])
                k.tt("pool", mim[:], tA[:], tB[:], ALU.subtract, reads=["tA", "tB"], writes=["mim"])
                P.op("dve", (lambda i: lambda e: e.tensor_tensor_scan(
                    out=qre[:], data0=mags[:, i:i + 1].to_broadcast([128, 256]), data1=mre[:],
                    initial=qi_re[:, i:i + 1], op0=ALU.mult, op1=ALU.add))(i),
                    reads=["mags", "mre", "qi"], writes=["qre"])
                P.op("dve", (lambda i: lambda e: e.tensor_tensor_scan(
                    out=qim[:], data0=mags[:, i:i + 1].to_broadcast([128, 256]), data1=mim[:],
                    initial=qi_im[:, i:i + 1], op0=ALU.mult, op1=ALU.add))(i),
                    reads=["mags", "mim", "qi"], writes=["qim"])
                a_ = qre[:, 255:256]
                b_ = qim[:, 255:256]
                k.tt("pool", hd[:, 0:1], a_, ROTC[:, i:i + 1], ALU.mult, reads=["qre", "rot"], writes=["hd"])
                k.tt("pool", hd[:, 1:2], b_, ROTS[:, i:i + 1], ALU.mult, reads=["qim", "rot"], writes=["hd"])
                k.tt("pool", hd[:, 2:3], a_, ROTS[:, i:i + 1], ALU.mult, reads=["qre", "rot"], writes=["hd"])
                k.tt("pool", hd[:, 3:4], b_, ROTC[:, i:i + 1], ALU.mult, reads=["qim", "rot"], writes=["hd"])
                k.tt("pool", qi_re[:, i:i + 1], hd[:, 0:1], hd[:, 1:2], ALU.subtract, reads=["hd", "qi"], writes=["qi"])
                k.tt("pool", qi_im[:, i:i + 1], hd[:, 2:3], hd[:, 3:4], ALU.add, reads=["hd", "qi"], writes=["qi"])
                k.tt("pool", PV[0][:], qre[:], Ct, ALU.mult, reads=["qre", "tabs"], writes=[("PV", 0)])
                k.tt("pool", PV[1][:], qim[:], St, ALU.mult, reads=["qim", "tabs"], writes=[("PV", 1)])
                k.tt("dve", PV[2][:], qre[:], St, ALU.mult, reads=["qre", "tabs"], writes=[("PV", 2)])
                k.tt("dve", PV[3][:], qim[:], Ct, ALU.mult, reads=["qim", "tabs"], writes=[("PV", 3)])
                for v in range(4):
                    k.mm(banks[c2][:, 0:256], CW[:, i, v, :], PV[v][:], i % 4 == 0 and v == 0, i % 4 == 3 and v == 3,
                         reads=["CW", ("PV", v)], writes=[("ACC", c2)])
            for c2 in range(2):
                k.tt("dve", yb[:], du[:, c2, cs_], banks[c2][:, 0:256], ALU.add, reads=["du", ("ACC", c2)], writes=["yb"])
                k.act(y2[:], yb[:], AF.Square, reads=["yb"], writes=["y2"])
                k.ts("pool", y2[:], y2[:], 0.0713548162726, ALU.mult, reads=["y2"], writes=["y2"], s2=1.59576912161,
                     op1=ALU.add)
                k.tt("pool", y2[:], y2[:], yb[:], ALU.mult, reads=["y2", "yb"], writes=["y2"])
                k.act(sg[:], y2[:], AF.Sigmoid, reads=["y2"], writes=["sg"])
                k.tt("pool", gT[:, c2, :], yb[:], sg[:], ALU.mult, reads=["yb", "sg"], writes=["gT"])
            for t2 in range(2):
                T_ = 4 * G + 2 * sb_ + t2
                gl, kgl = wb()
                for c2 in range(2):
                    k.mm(gl[:], gT[:, c2, t2 * 128:(t2 + 1) * 128], wglu[:, c2, :], c2 == 0, c2 == 1,
                         reads=["gT", "wglu"], writes=[kgl])
                k.act(sgt[:], gl[:, 256:512], AF.Sigmoid, reads=[kgl], writes=["sgt"])
                oc = OC[T_ % 2]
                k.tt("dve", oc[:], sgt[:], gl[:, 0:256], ALU.mult, reads=["sgt", kgl], writes=[("OC", T_ % 2)])
                P.dma("sp", o_d[T_ * 128:(T_ + 1) * 128, 512:768], oc[:], reads=[("OC", T_ % 2)], writes=[ko])


WNAMES = ("ln_g", "ln_b", "ffn_wi", "ffn_wo", "w_in", "w_branch", "w_out", "ret_gn", "mla_q_norm", "mla_kv_norm",
          "mla_w_uq", "mla_w_ukv")


def build_program(plan, ext=()):
    nc = bass.Bass("TRN2", target_bir_lowering=False)

    def din(name, shape, dtype=F32):
        return nc.dram_tensor(name, list(shape), dtype, kind="ExternalInput").ap()

    x = din("x", [SEQ, D])
    ln_g = din("ln_g", [DEPTH, 3, D])
    ln_b = din("ln_b", [DEPTH, 3, D])
    ffn_wi = din("ffn_wi", [DEPTH, 2, D, 2 * DFF])
    ffn_wo = din("ffn_wo", [DEPTH, 2, DFF, D])
    w_in = din("w_in", [DEPTH, D, NIN])
    w_branch = din("w_branch", [DEPTH, 4, 256, D])
    w_out = din("w_out", [DEPTH, D, D])
    ret_gn = din("ret_gn", [DEPTH, 256])
    mla_q_norm = din("mla_q_norm", [DEPTH, 256])
    mla_kv_norm = din("mla_kv_norm", [DEPTH, 128])
    mla_w_uq = din("mla_w_uq", [DEPTH, 256, 384])
    mla_w_ukv = din("mla_w_ukv", [DEPTH, 128, 512])
    s5_w_glu = din("s5_w_glu", [DEPTH, 256, 512])
    S5IN = {n: din(n, [DEPTH] + shp) for n, shp in S5_SHAPES.items()}
    CD = {}
    for nm, shp in CONST_SHAPES.items():
        CD[nm] = din(nm, shp)
    c_ident = CD["c_ident"]
    out = nc.dram_tensor("out", [SEQ, D], F32, kind="ExternalOutput").ap()
    S = {}
    for nm in ("s_x1", "s_o", "s_x2", "s_x3"):
        S[nm] = nc.dram_tensor(nm, [SEQ, D], F32, kind="ExternalInput" if nm in ext else "Internal").ap()

    P = Prog(nc)
    k = K(P)
    C = {}
    C["ident"] = P.sb([128, 128], F32)
    P.dma("sp", C["ident"][:], c_ident, writes=["ident"])
    P.persist()
    banks = [P.ps([128, 512], F32) for _ in range(6)]
    C["ACC"] = banks[0:4]
    C["PW"] = banks[4:6]
    C["banks"] = banks
    bbanks = [P.ps([128, 1024], BF16) for _ in range(2)]
    wc = [0]

    def wbank():
        i = 2 + wc[0] % 4
        wc[0] += 1
        return banks[i], ("ACC", i) if i < 4 else ("PW", i - 4)
    C["wbank"] = wbank
    bbc = [0]

    def bbank():
        i = bbc[0] % 2
        bbc[0] += 1
        return bbanks[i], ("BB", i)
    C["bbank"] = bbank
    bc = [0]

    def bank():
        i = bc[0] % 6
        bc[0] += 1
        return banks[i], ("ACC", i) if i < 4 else ("PW", i - 4)
    C["bank"] = bank

    nplan = len(plan)
    for pi, (st, l) in enumerate(plan):
        last = pi == nplan - 1
        if st == "ffn1":
            src, ks = (x, "x") if l == 0 else (S["s_x3"], "s_x3")
            dst, kd = (out, "out") if last else (S["s_x1"], "s_x1")
            ffn_stage(P, k, C, src, ks, dst, kd, ffn_wi[l, 0], ffn_wo[l, 0], ln_g[l, 0], ln_b[l, 0])
        elif st == "mixb":
            dst, kd = (out, "out") if last else (S["s_x2"], "s_x2")
            mixb_stage(P, k, C, S["s_x1"], "s_x1", S["s_o"], "s_o", dst, kd, w_in[l], w_branch[l], w_out[l],
                       ln_g[l, 1], ln_b[l, 1])
        elif st == "mixa1":
            od, kod = (out, "out") if last else (S["s_o"], "s_o")
            mixa1_stage(P, k, C, S["s_x1"], "s_x1", od, kod, w_in[l], CD)
        elif st in ("mixa2", "mixa2r", "mixa2m", "mixa2n"):
            od, kod = (out, "out") if last else (S["s_o"], "s_o")
            mixa2_stage(P, k, C, S["s_x1"], "s_x1", od, kod, w_in[l], CD, ret_gn[l], mla_q_norm[l], mla_kv_norm[l],
                        mla_w_uq[l], mla_w_ukv[l], do_ret=st in ("mixa2", "mixa2r"), do_mla=st in ("mixa2", "mixa2m"))
        elif st == "mixa3":
            od, kod = (out, "out") if last else (S["s_o"], "s_o")
            mixa3_stage(P, k, C, S["s_x1"], "s_x1", od, kod, w_in[l], CD, {n: a[l] for n, a in S5IN.items()}, s5_w_glu[l])
        elif st == "ffn2":
            dst, kd = (out, "out") if last else (S["s_x3"], "s_x3")
            ffn_stage(P, k, C, S["s_x2"], "s_x2", dst, kd, ffn_wi[l, 1], ffn_wo[l, 1], ln_g[l, 2], ln_b[l, 2])
        else:
            raise ValueError(st)
    P.finish()
    P.build()
    print("ops", P.n_ops, "sbuf max", P.sb_max, flush=True)
    return nc


S5_SHAPES = {
    "s5c_are": [128, 2, 64], "s5c_aim": [128, 2, 64], "s5c_bre": [128, 2, 64], "s5c_bim": [128, 2, 64],
    "s5c_ls": [128, 2], "s5c_d": [128, 2],
    "s5s_are": [128, 8], "s5s_aim": [128, 8], "s5s_ls": [128, 8],
    "s5s_cre": [128, 8, 16], "s5s_cim": [128, 8, 16],
}


def s5_layouts(inputs):
    f = lambda n: np.asarray(inputs[n], dtype=np.float32)
    a_re, a_im, b_re, b_im = f("s5_a_re"), f("s5_a_im"), f("s5_b_re"), f("s5_b_im")
    c_re, c_im, d_, ls = f("s5_c_re"), f("s5_c_im"), f("s5_d"), f("s5_log_step")
    L = a_re.shape[0]
    p = np.arange(128)
    out = {}
    gl, c = p // 16, p % 16
    g_c = (np.arange(2)[None, :] * 8 + gl[:, None])
    out["s5c_are"] = a_re[:, g_c, :]
    out["s5c_aim"] = a_im[:, g_c, :]
    out["s5c_bre"] = np.stack([b_re[:, g_c[:, j], :, c] for j in range(2)], axis=0).transpose(2, 1, 0, 3) \
        if False else np.stack([np.stack([b_re[l][g_c[:, j], :, c] for j in range(2)], axis=1) for l in range(L)], 0)
    out["s5c_bim"] = np.stack([np.stack([b_im[l][g_c[:, j], :, c] for j in range(2)], axis=1) for l in range(L)], 0)
    out["s5c_ls"] = ls[:, g_c]
    out["s5c_d"] = np.stack([np.stack([d_[l][g_c[:, j], c] for j in range(2)], axis=1) for l in range(L)], 0)
    gl2, s_ = p // 64, p % 64
    g_s = 2 * np.arange(8)[None, :] + gl2[:, None]
    out["s5s_are"] = np.stack([a_re[l][g_s, s_[:, None]] for l in range(L)], 0)
    out["s5s_aim"] = np.stack([a_im[l][g_s, s_[:, None]] for l in range(L)], 0)
    out["s5s_ls"] = ls[:, g_s]
    out["s5s_cre"] = np.stack([c_re[l][g_s, :, s_[:, None]] for l in range(L)], 0)
    out["s5s_cim"] = np.stack([c_im[l][g_s, :, s_[:, None]] for l in range(L)], 0)
    return {k_: np.ascontiguousarray(v.astype(np.float32)) for k_, v in out.items()}


CONST_SHAPES = {
    "c_ident": [128, 128],
    "c_kpos": [2, SEQ],
    "c_qaug": [4, 2, 512],
    "c_bq": [128, 32, 4],
    "c_corr": [128, 4, 128],
    "c_pw2": [128, 2, NIT],
    "c_negidx": [128, SEQ],
    "c_qpos": [128, 32],
    "c_nsl": [128, 4],
    "c_xi": [128, 2, 512],
    "c_zeta": [128, 256],
    "c_dect": [128, 512],
    "c_gch": [128, 128],
    "c_cmt": [128, 512],
    "c_cos": [128, 32, 16],
    "c_sin": [128, 32, 16],
    "c_msk2": [128, 2],
    "c_rm": [128, 4],
    "c_iota": [128, 256],
}


def make_consts():
    c = {}
    c["c_ident"] = np.eye(128, dtype=np.float32)
    pos = np.arange(SEQ)
    c["c_kpos"] = np.stack([pos // 64, pos % 64]).astype(np.float32)
    qa = np.zeros((4, 2, 512), np.float32)
    for h in range(4):
        qa[h, 0, :] = 512.0 * SLOPES[h]
        qa[h, 1, :] = 8.0 * SLOPES[h]
    c["c_qaug"] = qa
    p = np.arange(128)[:, None, None]
    T = np.arange(32)[None, :, None]
    sl = np.array(SLOPES, np.float64)[None, None, :]
    c["c_bq"] = (-(sl) * (128 * T + p)).astype(np.float32)
    qq = np.arange(128)[:, None]
    kk = np.arange(128)[None, :]
    corr = np.ones((128, 4, 128), np.float64)
    for h in range(4):
        corr[:, h, :] = np.where(kk > qq, -2.0 * SLOPES[h] * (kk - qq), 0.0)
    c["c_corr"] = corr.astype(np.float32)
    pw = np.zeros((128, 2, NIT), np.float32)
    for it in range(NIT):
        pw[:, 0, it] = 2.0 ** (-(it + 1))
        pw[:, 1, it] = -(2.0 ** (-(it + 2)))
    c["c_pw2"] = pw
    lg = np.log1p(-np.exp2(-5.0 - np.arange(4, dtype=np.float64)))
    rows = np.arange(128)
    xi = np.zeros((128, 2, 512))
    for t in range(2):
        hh = 2 * t + rows // 64
        n = np.arange(512) % 128
        xi[:, t, :] = np.exp(lg[hh][:, None] * (n[None, :] + 1.0)) / 8.0
    c["c_xi"] = xi.astype(np.float32)
    m = rows
    zeta = np.zeros((128, 256))
    dect = np.zeros((128, 512))
    nn = np.arange(128)
    same = (m[:, None] // 64) == (nn[None, :] // 64)
    cross = (m[:, None] < 64) & (nn[None, :] >= 64)
    for h in range(4):
        zeta[:, 64 * h:64 * h + 64] = np.exp(lg[h] * (127.0 - m))[:, None]
        dd = np.where(same, np.exp(lg[h] * np.abs(nn[None, :] - m[:, None])), 0.0)
        dd = np.where(cross, np.exp(lg[h] * (nn[None, :] - m[:, None])), dd)
        dect[:, 128 * h:128 * h + 128] = dd / 8.0
    c["c_zeta"] = zeta.astype(np.float32)
    c["c_dect"] = dect.astype(np.float32)
    gch = np.zeros((128, 128))
    for t in range(2):
        hh = 2 * t + rows // 64
        gch[:, 64 * t:64 * t + 64] = np.exp(lg[hh] * 128.0)[:, None]
    c["c_gch"] = gch.astype(np.float32)
    kq = np.arange(128)
    cm = np.ones((128, 128), np.float32)
    cm[64:, :64] = 0.0
    c["c_cmt"] = np.tile(cm, (1, 4))
    pos = (128.0 * np.arange(32)[None, :] + np.arange(128)[:, None])
    fr = 10000.0 ** (-np.arange(16, dtype=np.float64) / 16.0)
    ang = (pos[:, :, None].astype(np.float32) * fr[None, None, :].astype(np.float32)).astype(np.float32)
    c["c_cos"] = np.cos(ang).astype(np.float32)
    c["c_sin"] = np.sin(ang).astype(np.float32)
    pp = np.arange(128)
    c["c_msk2"] = np.stack([((pp // 16) % 2 == 0), ((pp // 16) % 2 == 1)], axis=1).astype(np.float32)
    c["c_rm"] = (pp[:, None] // 32 == np.arange(4)[None, :]).astype(np.float32)
    c["c_iota"] = np.broadcast_to(np.arange(256, dtype=np.float32)[None, :], (128, 256)).copy()
    c["c_qpos"] = (128.0 * np.arange(32)[None, :] + np.arange(128)[:, None]).astype(np.float32)
    c["c_nsl"] = np.broadcast_to(-np.array(SLOPES, np.float32)[None, :], (128, 4)).copy()
    c["c_negidx"] = np.broadcast_to(-(np.arange(SEQ, dtype=np.float32) * np.float32(2.0 ** -12))[None, :], (128, SEQ)).copy()
    return c


def make_shared(inputs):
    shared = {}
    for name in WNAMES:
        shared[name] = np.ascontiguousarray(np.asarray(inputs[name], dtype=np.float32))
    shared["s5_w_glu"] = np.ascontiguousarray(np.asarray(inputs["s5_w_glu"], dtype=np.float32))
    shared.update(s5_layouts(inputs))
    shared.update(make_consts())
    return shared


FULL_PLAN = [(st, l) for l in range(DEPTH) for st in ("ffn1", "mixa1", "mixa2", "mixa3", "mixb", "ffn2")]


def kernel(**inputs):
    ncores = 8
    nc = build_program(FULL_PLAN)
    xs = np.ascontiguousarray(np.asarray(inputs["x"], dtype=np.float32))
    shared = make_shared(inputs)
    in_maps = []
    for c in range(ncores):
        m = dict(shared)
        m["x"] = xs[c % 4]
        in_maps.append(m)
    res = run_bass_kernel_spmd(nc, in_maps, core_ids=list(range(ncores)))
    outs = [res.results[c]["out"] for c in range(4)]
    return np.stack(outs, axis=0).astype(np.float32)
```
